# Optimizing a Trainium2 kernel written in Bass

```python
import math
import jax, jax.numpy as jnp
from jax import lax
import numpy as np

D_MODEL = 1024
BATCH = 8
SEQ = 4096
DEPTH = 1

HEAD_DIM = 64
FOX_HEADS = 8
DIL_HEADS = 8
FOX_W = FOX_HEADS * HEAD_DIM
DIL_W = DIL_HEADS * HEAD_DIM
Q_BLOCK = 128
DILATED_PATTERNS = ((128, 1), (512, 4), (2048, 16))
ROPE_THETA = 500000.0
ROT_DIM = HEAD_DIM // 4
N_GROUPS = 4
EXPERTS_PER_GROUP = 8
N_EXPERTS = N_GROUPS * EXPERTS_PER_GROUP
TOP_K = 2
D_EXPERT = 512
MOE_BLOCK = 256
LN_EPS = 1e-5
NEG = -1e30
DEEPNORM_ALPHA = (2 * DEPTH) ** 0.25
DEEPNORM_BETA = (8 * DEPTH) ** (-0.25)
PROJ_SIZES = (FOX_W, FOX_W, FOX_W, FOX_W, FOX_HEADS, DIL_W, DIL_W, DIL_W)
PROJ_COLS = sum(PROJ_SIZES)

kernel_name = "hybrid_fox_dilated_hmoe_deepnorm_layer"


def layer_norm(x, g, b):
    xf = x.astype(jnp.float32)
    mu = jnp.mean(xf, axis=-1, keepdims=True)
    var = jnp.mean(jnp.square(xf - mu), axis=-1, keepdims=True)
    return ((xf - mu) * lax.rsqrt(var + LN_EPS) * g + b).astype(x.dtype)


def partial_rope(t, positions):
    half = ROT_DIM // 2
    inv_freq = ROPE_THETA ** (-jnp.arange(0, ROT_DIM, 2, dtype=jnp.float32) / ROT_DIM)
    ang = positions.astype(jnp.float32)[..., None] * inv_freq
    cos, sin = jnp.cos(ang)[:, :, None, :], jnp.sin(ang)[:, :, None, :]
    tf = t.astype(jnp.float32)
    x1, x2, rest = tf[..., :half], tf[..., half:ROT_DIM], tf[..., ROT_DIM:]
    out = jnp.concatenate([x1 * cos - x2 * sin, x2 * cos + x1 * sin, rest], axis=-1)
    return out.astype(t.dtype)


def forgetting_attention(q, k, v, log_f):
    B, H, S, hd = q.shape
    nb = S // Q_BLOCK
    cum = jnp.cumsum(log_f, axis=-1)
    qb = q.reshape(B, H, nb, Q_BLOCK, hd).transpose(2, 0, 1, 3, 4)
    cb = cum.reshape(B, H, nb, Q_BLOCK).transpose(2, 0, 1, 3)
    kpos = jnp.arange(S)
    scale = HEAD_DIM ** -0.5

    def block(args):
        qi, ci, n = args
        s = jnp.einsum('bhqd,bhkd->bhqk', qi, k) * scale + ci[..., :, None] - cum[:, :, None, :]
        qpos = n * Q_BLOCK + jnp.arange(Q_BLOCK)
        s = jnp.where(kpos[None, :] <= qpos[:, None], s, NEG)
        p = jax.nn.softmax(s, axis=-1)
        return jnp.einsum('bhqk,bhkd->bhqd', p, v)

    o = lax.map(block, (qb, cb, jnp.arange(nb)))
    return o.transpose(1, 2, 0, 3, 4).reshape(B, H, S, hd)


def dilated_window_attention(q, k, v, window, dilation):
    B, H, S, hd = q.shape
    w = window // dilation
    unit = dilation * w
    Sp = -(-S // unit) * unit
    L = Sp // dilation
    nb = L // w
    pad = ((0, 0), (0, 0), (0, Sp - S), (0, 0))

    def to_res(a):
        a = jnp.pad(a, pad).reshape(B, H, L, dilation, hd).transpose(0, 1, 3, 2, 4)
        return a.reshape(B, H, dilation, nb, w, hd)

    def with_prev(a):
        prev = jnp.pad(a, ((0, 0), (0, 0), (0, 0), (1, 0), (0, 0), (0, 0)))[:, :, :, :nb]
        return jnp.concatenate([prev, a], axis=4)

    qr = to_res(q)
    kr = with_prev(to_res(k))
    vr = with_prev(to_res(v))
    s = jnp.einsum('bhrnqd,bhrnkd->bhrnqk', qr, kr) * (HEAD_DIM ** -0.5)
    n_i = jnp.arange(nb)[:, None, None]
    i_i = jnp.arange(w)[None, :, None]
    j_i = jnp.arange(2 * w)[None, None, :]
    valid = (j_i >= i_i) & (j_i <= i_i + w) & ((n_i > 0) | (j_i >= w))
    s = jnp.where(valid, s, NEG)
    lse = jax.nn.logsumexp(s, axis=-1)
    p = jnp.exp(s - lse[..., None])
    o = jnp.einsum('bhrnqk,bhrnkd->bhrnqd', p, vr)
    o = o.reshape(B, H, dilation, L, hd).transpose(0, 1, 3, 2, 4).reshape(B, H, Sp, hd)[:, :, :S]
    lse = lse.reshape(B, H, dilation, L).transpose(0, 1, 3, 2).reshape(B, H, Sp)[:, :, :S]
    return o, lse


def mixing_sublayer(h, positions, w_in, b_forget, w_out):
    B, S, _ = h.shape
    proj = h @ w_in
    splits = list(np.cumsum(PROJ_SIZES)[:-1])
    fq, fk, fv, fog, ff, dq, dk, dv = jnp.split(proj, splits, axis=-1)

    def heads(t, n):
        return t.reshape(B, S, n, HEAD_DIM)

    def bhsd(t):
        return t.transpose(0, 2, 1, 3).astype(jnp.float32)

    log_f = jax.nn.log_sigmoid(ff.astype(jnp.float32) + b_forget).transpose(0, 2, 1)
    fox = forgetting_attention(bhsd(heads(fq, FOX_HEADS)), bhsd(heads(fk, FOX_HEADS)),
                               bhsd(heads(fv, FOX_HEADS)), log_f)
    fox = fox.transpose(0, 2, 1, 3).reshape(B, S, FOX_W) * jax.nn.sigmoid(fog.astype(jnp.float32))

    dqh = bhsd(partial_rope(heads(dq, DIL_HEADS), positions))
    dkh = bhsd(partial_rope(heads(dk, DIL_HEADS), positions))
    dvh = bhsd(heads(dv, DIL_HEADS))
    outs, lses = [], []
    for window, dilation in DILATED_PATTERNS:
        o, l = dilated_window_attention(dqh, dkh, dvh, window, dilation)
        outs.append(o)
        lses.append(l)
    wts = jax.nn.softmax(jnp.stack(lses, axis=0), axis=0)
    dil = jnp.sum(wts[..., None] * jnp.stack(outs, axis=0), axis=0)
    dil = dil.transpose(0, 2, 1, 3).reshape(B, S, DIL_W)

    merged = jnp.concatenate([fox, dil], axis=-1).astype(h.dtype)
    return merged @ w_out


def hierarchical_moe(h, w_rg, b_rg, w_re, b_re, w_up, w_gate, w_down):
    N, D = h.shape
    gl = (h @ w_rg).astype(jnp.float32) + b_rg
    g = jnp.argmax(gl, axis=-1)
    p_g = jnp.take_along_axis(jax.nn.softmax(gl, axis=-1), g[:, None], axis=-1)[:, 0]
    el = ((h @ w_re).astype(jnp.float32) + b_re).reshape(N, N_GROUPS, EXPERTS_PER_GROUP)
    el_g = jnp.take_along_axis(el, g[:, None, None], axis=1)[:, 0]
    top_v, top_i = lax.top_k(el_g, TOP_K)
    top_w = jax.nn.softmax(top_v, axis=-1) * p_g[:, None]

    A = N * TOP_K
    eid = (g[:, None] * EXPERTS_PER_GROUP + top_i).reshape(A).astype(jnp.int32)
    tok = jnp.repeat(jnp.arange(N, dtype=jnp.int32), TOP_K)
    wgt = top_w.reshape(A)
    order = jnp.argsort(eid)
    s_eid, s_tok, s_w = eid[order], tok[order], wgt[order]
    counts = jnp.bincount(eid, length=N_EXPERTS)
    pcounts = (counts + MOE_BLOCK - 1) // MOE_BLOCK * MOE_BLOCK
    offs = jnp.cumsum(counts) - counts
    pend = jnp.cumsum(pcounts)
    poffs = pend - pcounts
    dest = poffs[s_eid] + (jnp.arange(A) - offs[s_eid])
    nblk = -(-A // MOE_BLOCK) + N_EXPERTS
    P = nblk * MOE_BLOCK
    row_tok = jnp.full((P,), N, dtype=jnp.int32).at[dest].set(s_tok)
    row_w = jnp.zeros((P,), dtype=h.dtype).at[dest].set(s_w.astype(h.dtype))
    blk_e = jnp.minimum(jnp.searchsorted(pend, jnp.arange(nblk) * MOE_BLOCK, side='right'),
                        N_EXPERTS - 1)
    xr = h[jnp.minimum(row_tok, N - 1)].reshape(nblk, MOE_BLOCK, D)

    def expert_block(args):
        xb, e = args
        return (jax.nn.silu(xb @ w_gate[e]) * (xb @ w_up[e])) @ w_down[e]

    yr = lax.map(expert_block, (xr, blk_e)).reshape(P, D)
    return jax.ops.segment_sum(yr * row_w[:, None], row_tok, num_segments=N)


def setup_inputs(seed: int = 0) -> dict:
    key = jax.random.key(seed)
    ks = jax.random.split(key, 20)
    D = D_MODEL
    f32 = jnp.float32
    x = jax.random.normal(ks[0], (BATCH, SEQ, D), f32)
    c = jax.random.normal(ks[1], (BATCH, D), f32)
    positions = (jax.random.randint(ks[2], (BATCH, 1), 0, 2048)
                 + jnp.arange(SEQ)[None, :]).astype(jnp.int32)
    w_ada = jax.random.normal(ks[3], (D, 6 * D), f32) * (0.5 * D ** -0.5)
    b_ada = jax.random.normal(ks[4], (6 * D,), f32) * 0.01
    col_scale = np.ones((PROJ_COLS,), np.float32)
    bounds = np.concatenate([[0], np.cumsum(PROJ_SIZES)])
    for idx in (2, 7):
        col_scale[bounds[idx]:bounds[idx + 1]] = DEEPNORM_BETA
    w_in = jax.random.normal(ks[5], (D, PROJ_COLS), f32) * (D ** -0.5) * jnp.asarray(col_scale)
    b_forget = 2.0 + 4.0 * jax.random.uniform(ks[6], (FOX_HEADS,), f32)
    w_out = jax.random.normal(ks[7], (D, D), f32) * (D ** -0.5) * DEEPNORM_BETA
    ln1_g = 1.0 + 0.01 * jax.random.normal(ks[8], (D,), f32)
    ln1_b = 0.01 * jax.random.normal(ks[9], (D,), f32)
    w_router_group = jax.random.normal(ks[10], (D, N_GROUPS), f32) * (D ** -0.5)
    b_router_group = 0.01 * jax.random.normal(ks[11], (N_GROUPS,), f32)
    w_router_expert = jax.random.normal(ks[12], (D, N_EXPERTS), f32) * (D ** -0.5)
    b_router_expert = 0.01 * jax.random.normal(ks[13], (N_EXPERTS,), f32)
    w_up = jax.random.normal(ks[14], (N_EXPERTS, D, D_EXPERT), f32) * (D ** -0.5)
    w_gate = jax.random.normal(ks[15], (N_EXPERTS, D, D_EXPERT), f32) * (D ** -0.5)
    w_down = jax.random.normal(ks[16], (N_EXPERTS, D_EXPERT, D), f32) * (D_EXPERT ** -0.5) * DEEPNORM_BETA
    ln2_g = 1.0 + 0.01 * jax.random.normal(ks[17], (D,), f32)
    ln2_b = 0.01 * jax.random.normal(ks[18], (D,), f32)
    return {"x": x, "c": c, "positions": positions, "w_ada": w_ada, "b_ada": b_ada,
            "w_in": w_in, "b_forget": b_forget, "w_out": w_out, "ln1_g": ln1_g, "ln1_b": ln1_b,
            "w_router_group": w_router_group, "b_router_group": b_router_group,
            "w_router_expert": w_router_expert, "b_router_expert": b_router_expert,
            "w_up": w_up, "w_gate": w_gate, "w_down": w_down, "ln2_g": ln2_g, "ln2_b": ln2_b}


def reference(x, c, positions, w_ada, b_ada, w_in, b_forget, w_out, ln1_g, ln1_b,
              w_router_group, b_router_group, w_router_expert, b_router_expert,
              w_up, w_gate, w_down, ln2_g, ln2_b):
    B, S, D = x.shape
    mod = jax.nn.silu(c) @ w_ada + b_ada
    shift1, scale1, gate1, shift2, scale2, gate2 = jnp.split(mod, 6, axis=-1)
    for _ in range(DEPTH):
        h = x * (1.0 + scale1[:, None, :]) + shift1[:, None, :]
        y = mixing_sublayer(h, positions, w_in, b_forget, w_out)
        x = layer_norm(DEEPNORM_ALPHA * x + gate1[:, None, :] * y, ln1_g, ln1_b)
        h = x * (1.0 + scale2[:, None, :]) + shift2[:, None, :]
        y = hierarchical_moe(h.reshape(B * S, D), w_router_group, b_router_group,
                             w_router_expert, b_router_expert, w_up, w_gate, w_down).reshape(B, S, D)
        x = layer_norm(DEEPNORM_ALPHA * x + gate2[:, None, :] * y, ln2_g, ln2_b)
    return x
```

```python
from contextlib import ExitStack
import math
import numpy as np
import ml_dtypes
import concourse.bass as bass
import concourse.mybir as mybir
from concourse.bass_utils import run_bass_kernel_spmd
from concourse.alu_op_type import AluOpType as ALU

F32 = mybir.dt.float32
BF16 = mybir.dt.bfloat16
I32 = mybir.dt.int32
AF = mybir.ActivationFunctionType
AX = mybir.AxisListType

S_LEN = 4096
D = 1024
NT = 32
ALPHA = 2.0 ** 0.25
LN_EPS = 1e-5
NEXP = 32
RCAP = [1152, 896, 768, 768, 640, 640, 640, 640] + [512] * 7 + [384] * 14 + [256] * 3
ROFF = [sum(RCAP[:i]) for i in range(NEXP)]
TOTSLOT = sum(RCAP)


class Buf:
    __slots__ = ("name", "w", "r", "dsem", "dcnt")

    def __init__(self, name):
        self.name = name
        self.w = None
        self.r = []
        self.dsem = None
        self.dcnt = 0


class Sched:
    ENG = ("pe", "act", "dve", "pool", "sp")

    def __init__(self, nc, stack):
        self.nc = nc
        self.stack = stack
        self.prog = {e: [] for e in self.ENG}
        self.sems = {}
        self.cnt = {e: 0 for e in self.ENG}
        self.seen = {e: {} for e in self.ENG}
        for e in self.ENG:
            self.sems[e] = stack.enter_context(nc.semaphore("s_" + e))
        self.nsem = len(self.ENG)
        self.dpool = {}

    def buf(self, name, dgroup=None):
        b = Buf(name)
        if dgroup is not None:
            b.dsem = self._mk(dgroup)
        return b

    def _mk(self, key):
        key = "d_" + key
        if key not in self.sems:
            self.sems[key] = self.stack.enter_context(self.nc.semaphore(key))
            self.nsem += 1
            self.dpool[key] = 0
        return key

    def _dsem(self, b):
        if b.dsem is None:
            b.dsem = self._mk(b.name)
        return b.dsem

    def _waits(self, e, reads, writes):
        toks = []
        for b in reads:
            if b.w is not None:
                toks.append(b.w)
        for b in writes:
            if b.w is not None:
                toks.append(b.w)
            toks.extend(b.r)
        best = {}
        for (k, v) in toks:
            if e == "pe" and k == "pe":
                continue
            if v > best.get(k, 0):
                best[k] = v
        out = []
        for k, v in best.items():
            if self.seen[e].get(k, 0) >= v:
                continue
            self.seen[e][k] = v
            out.append((k, v))
        return out

    def op(self, e, fn, reads=(), writes=()):
        waits = self._waits(e, reads, writes)
        self.cnt[e] += 1
        tok = (e, self.cnt[e])
        self.prog[e].append((waits, fn, (e, 1)))
        for b in reads:
            b.r.append(tok)
            if len(b.r) > 64:
                b.r = b.r[-48:]
        for b in writes:
            b.w = tok
            b.r = []
        return tok

    def dma(self, q, fn, reads=(), writes=(), semb=None):
        waits = self._waits(q, reads, writes)
        sb = semb if semb is not None else (writes[0] if writes else reads[0])
        key = self._dsem(sb)
        self.dpool[key] += 16
        tok = (key, self.dpool[key])
        self.prog[q].append((waits, fn, (key, 16)))
        for b in reads:
            b.r.append(tok)
        for b in writes:
            b.w = tok
            b.r = []
        return tok

    def barrier(self):
        toks = [(e, self.cnt[e]) for e in self.ENG if self.cnt[e] > 0]
        toks += [(k, v) for k, v in self.dpool.items() if v > 0]
        for e in self.ENG:
            waits = []
            for (k, v) in toks:
                if self.seen[e].get(k, 0) >= v:
                    continue
                self.seen[e][k] = v
                waits.append((k, v))
            if waits:
                self.prog[e].append((waits, None, None))

    def final_wait(self, e, bufs):
        waits = self._waits(e, bufs, bufs)
        self.prog[e].append((waits, None, None))

    def emit(self, block):
        sems = self.sems
        prog = self.prog

        def run(eng, lst):
            for waits, fn, inc in lst:
                for (k, v) in waits:
                    eng.wait_ge(sems[k], v)
                if fn is not None:
                    ins = fn(eng)
                    ins.then_inc(sems[inc[0]], inc[1])

        @block.tensor
        def _(eng):
            run(eng, prog["pe"])

        @block.scalar
        def _(eng):
            run(eng, prog["act"])

        @block.vector
        def _(eng):
            run(eng, prog["dve"])

        @block.gpsimd
        def _(eng):
            run(eng, prog["pool"])

        @block.sync
        def _(eng):
            run(eng, prog["sp"])


def host_consts():
    c = {}
    c["ident_f"] = np.eye(128, dtype=np.float32)
    c["ident_b"] = np.eye(128, dtype=np.float32).astype(ml_dtypes.bfloat16)
    j = np.arange(128)
    c["negU"] = -(j[:, None] <= j[None, :]).astype(np.float32)
    c["negOnes"] = -np.ones((128, 128), np.float32)
    c["ustrict"] = (j[:, None] < j[None, :]).astype(np.float32)
    c["ones_f"] = np.ones((128, 128), np.float32)
    mc = (j[:, None] <= j[None, :]).astype(np.float32)
    mp = (j[:, None] >= j[None, :]).astype(np.float32)
    c["mask4"] = np.concatenate([mp, mc, mp, mc], axis=1).astype(ml_dtypes.bfloat16)
    inv = (500000.0 ** (-np.arange(0, 16, 2, dtype=np.float32) / 16.0)).astype(np.float32)
    invf = np.zeros((128, 1), np.float32)
    rot = np.zeros((128, 128), np.float32)
    for hh in range(2):
        for i in range(16):
            invf[hh * 64 + i, 0] = inv[i % 8]
        for i in range(8):
            rot[hh * 64 + i + 8, hh * 64 + i] = -1.0
            rot[hh * 64 + i, hh * 64 + i + 8] = 1.0
    c["invf"] = invf
    bc = lambda v: np.broadcast_to(np.asarray(v, np.float32)[None, :], (128, len(v))).copy()
    c["roff"] = bc(ROFF)
    c["rcap1"] = bc([v - 1 for v in RCAP])
    c["riota"] = bc(np.arange(NEXP))
    p = np.arange(128, dtype=np.float32)[:, None]
    c["iotaU"] = (np.arange(8, dtype=np.float32)[None, :] * 128 + p).astype(np.float32)
    c["iotaD"] = (np.arange(4, dtype=np.float32)[None, :] * 128 + p).astype(np.float32)
    c["rotm"] = rot.astype(ml_dtypes.bfloat16)
    return c


CONST_SPECS = {
    "ident_f": ([128, 128], F32), "ident_b": ([128, 128], BF16), "negU": ([128, 128], F32),
    "negOnes": ([128, 128], F32), "ustrict": ([128, 128], F32), "ones_f": ([128, 128], F32),
    "mask4": ([128, 512], BF16), "invf": ([128, 1], F32), "rotm": ([128, 128], BF16), "roff": ([128, 32], F32), "rcap1": ([128, 32], F32),
    "riota": ([128, 32], F32), "iotaU": ([128, 8], F32), "iotaD": ([128, 4], F32),
}


def build_nc(stop_after=99, debug=False):
    nc = bass.Bass("TRN2", target_bir_lowering=False)
    din = lambda n, s, d=F32: nc.dram_tensor(n, s, d, kind="ExternalInput").ap()
    x_d = din("x", [S_LEN, D])
    c_d = din("c", [128, 8])
    pos_d = din("positions", [1, S_LEN], I32)
    wada_d = din("w_ada", [D, 6 * D])
    bada_d = din("b_ada", [1, 6 * D])
    badaT_d = din("b_adaT", [128, 48])
    win_d = din("w_in", [D, 3592])
    bf_d = din("b_forget", [1, 8])
    wout_d = din("w_out", [D, D])
    ln1g_d = din("ln1_g", [1, D]); ln1b_d = din("ln1_b", [1, D])
    wrg_d = din("w_router_group", [D, 4]); brg_d = din("b_router_group", [1, 4])
    wre_d = din("w_router_expert", [D, 32]); bre_d = din("b_router_expert", [1, 32])
    wug_d = din("w_upgate_r", [NEXP * 128, 8 * 1024])
    wdn_d = din("w_down_r", [NEXP * 128, 4 * 1024])
    ln2g_d = din("ln2_g", [1, D]); ln2b_d = din("ln2_b", [1, D])
    cst_d = {k: din(k, s, d) for k, (s, d) in CONST_SPECS.items()}
    out_d = nc.dram_tensor("out", [S_LEN, D], F32, kind="ExternalOutput").ap()
    dbg_d = nc.dram_tensor("dbg", [128, 8 * S_LEN], F32, kind="ExternalOutput").ap() if debug else None
    scr = lambda n, s, d: nc.dram_tensor(n, s, d, kind="Internal").ap()
    mT_d = scr("mT_scr", [8, 128, S_LEN], BF16)
    G_d = scr("G_scr", [4, 128, S_LEN], BF16)
    vd_d = scr("vd_scr", [4, S_LEN, 2, 65], BF16)

    X1_d = scr("x1_scr", [S_LEN, D], F32)
    H2_d = scr("h2_scr", [S_LEN, D], BF16)
    Xe_d = scr("xe_scr", [TOTSLOT, D], BF16)
    Ys_d = scr("ys_scr", [TOTSLOT, D], BF16)
    win_v = win_d.rearrange("(kc p) c -> p kc c", p=128)

    with ExitStack() as st:
        S = Sched(nc, st)
        sb = lambda n, s, d: st.enter_context(nc.sbuf_tensor(n, s, d))
        PB = [st.enter_context(nc.psum_tensor("pb%d" % i, [128, 512], F32)) for i in range(8)]
        PBb = [S.buf("pb%d" % i) for i in range(8)]
        cst = {}
        cstb = {}
        for k, (s, d) in CONST_SPECS.items():
            cst[k] = sb("c_" + k, s, d)
            cstb[k] = S.buf("c_" + k, dgroup="consts")
            S.dma("sp", lambda e, k=k: e.dma_start(out=cst[k][:], in_=cst_d[k]), writes=[cstb[k]])
        for k in CONST_SPECS:
            cstb[k].w = ("d_consts", S.dpool["d_consts"])
        bout = S.buf("outdram")
        bdbg = S.buf("dbgdram")

        def dump(tile_ap, b, ncols, part=128, col0=0):
            if not debug:
                return
            S.dma("pool", lambda e: e.dma_start(out=dbg_d[0:part, col0:col0 + ncols], in_=tile_ap), reads=[b], writes=[bdbg])

        cT = sb("cT", [128, 8], F32); bcT = S.buf("cT")
        sc = sb("sc", [128, 8], F32); bsc = S.buf("sc")
        scB = sb("scB", [128, 8, 128], F32); bscB = S.buf("scB")
        badaT = sb("badaT", [128, 48], F32); bbadaT = S.buf("badaT")
        modF = sb("modF", [128, 16], F32); bmodF = S.buf("modF")
        g1b = sb("g1b", [128, D], F32); bg1b = S.buf("g1b")
        sh2b = sb("sh2b", [128, D], F32); bsh2b = S.buf("sh2b")
        s2b = sb("s2b", [128, D], F32); bs2b = S.buf("s2b")
        g2b = sb("g2b", [128, D], F32); bg2b = S.buf("g2b")
        S.dma("sp", lambda e: e.dma_start(out=cT[:], in_=c_d), writes=[bcT])
        S.dma("sp", lambda e: e.dma_start(out=badaT[:], in_=badaT_d), writes=[bbadaT])
        S.op("act", lambda e: e.activation(out=sc[:], in_=cT[:], func=AF.Silu), reads=[bcT], writes=[bsc])
        S.op("dve", lambda e: e.tensor_copy(out=scB[:], in_=sc[:].unsqueeze(2).broadcast_to([128, 8, 128])), reads=[bsc], writes=[bscB])
        stA = ExitStack()
        sbA = lambda n, s_, d: stA.enter_context(nc.sbuf_tensor(n, s_, d))
        hT = sbA("hT", [128, 8, S_LEN], BF16)
        bhT = [S.buf("hT%d" % tg) for tg in range(8)]
        with ExitStack() as st0:
            sb0 = lambda n, s, d: st0.enter_context(nc.sbuf_tensor(n, s, d))
            wa = [sb0("wa%d" % i, [128, 8, 512], F32) for i in range(2)]
            bwa = [S.buf("wa%d" % i) for i in range(2)]
            bb = [sb0("bb%d" % i, [128, 512], F32) for i in range(2)]
            bbb = [S.buf("bb%d" % i) for i in range(2)]
            xt = [sb0("xt%d" % i, [128, 4, D], F32) for i in range(2)]
            bxt = [S.buf("xt%d" % i) for i in range(2)]
            wada_v = wada_d.rearrange("(kc p) c -> p kc c", p=128)
            pieces = []
            for i in range(4):
                pieces.append((i * 512, "F", i))
            for vi, dest in ((2, (g1b, bg1b)), (3, (sh2b, bsh2b)), (4, (s2b, bs2b)), (5, (g2b, bg2b))):
                for hf in range(2):
                    pieces.append((vi * 1024 + hf * 512, "B", (dest, hf, vi)))

            def do_piece(pi):
                col0, kind, dest = pieces[pi]
                slot = pi % 2
                q = "sp" if pi % 2 == 0 else "pool"
                for kc4 in range(2):
                    S.dma(q, lambda e, kc4=kc4: e.dma_start(
                        out=wa[slot][:, kc4 * 4:(kc4 + 1) * 4, :], in_=wada_v[:, kc4 * 4:(kc4 + 1) * 4, col0:col0 + 512]), writes=[bwa[slot]])
                pbi = 4 + pi % 2
                if kind == "F":
                    for cc in range(4):
                        col = dest * 4 + cc
                        for kc in range(8):
                            S.op("pe", lambda e, cc=cc, kc=kc, col=col: e.matmul(
                                PB[pbi][:, col:col + 1], lhsT=wa[slot][:, kc, cc * 128:(cc + 1) * 128], rhs=sc[:, kc:kc + 1],
                                start=(kc == 0), stop=(kc == 7)), reads=[bwa[slot], bsc], writes=[PBb[pbi]])
                    S.op("dve", lambda e: e.tensor_tensor(
                        out=modF[:, dest * 4:dest * 4 + 4], in0=PB[pbi][:, dest * 4:dest * 4 + 4], in1=badaT[:, dest * 4:dest * 4 + 4], op=ALU.add),
                        reads=[PBb[pbi], bbadaT], writes=[bmodF])
                else:
                    (dt_, db_), hf, vi = dest
                    S.dma(q, lambda e: e.dma_start(out=bb[slot][:], in_=bada_d[0:1, col0:col0 + 512].broadcast_to([128, 512])), writes=[bbb[slot]])
                    for kc in range(8):
                        S.op("pe", lambda e, kc=kc: e.matmul(
                            PB[pbi][:, :], lhsT=scB[:, kc, :], rhs=wa[slot][:, kc, :], start=(kc == 0), stop=(kc == 7)),
                            reads=[bwa[slot], bscB], writes=[PBb[pbi]])
                    S.op("dve", lambda e: e.tensor_tensor(
                        out=dt_[:, hf * 512:(hf + 1) * 512], in0=PB[pbi][:, :], in1=bb[slot][:], op=ALU.add),
                        reads=[PBb[pbi], bbb[slot]], writes=[db_])

            for pi in range(4):
                do_piece(pi)
            S.op("dve", lambda e: e.tensor_scalar_add(out=modF[:, 8:16], in0=modF[:, 8:16], scalar1=1.0), reads=[bmodF], writes=[bmodF])

            for tg in range(8):
                sl = tg % 2
                xv = x_d[tg * 512:(tg + 1) * 512, :].rearrange("(j p) f -> p j f", p=128)
                for j2 in range(2):
                    S.dma("sp", lambda e, sl=sl, xv=xv, j2=j2: e.dma_start(
                        out=xt[sl][:, j2 * 2:(j2 + 1) * 2, :], in_=xv[:, j2 * 2:(j2 + 1) * 2, :]), writes=[bxt[sl]])
                if stop_after >= 1:
                    for fc in range(8):
                        pbi = (tg * 8 + fc) % 4
                        for j in range(4):
                            S.op("pe", lambda e, sl=sl, fc=fc, j=j, pbi=pbi: e.transpose(
                                out=PB[pbi][:, j * 128:(j + 1) * 128], in_=xt[sl][:, j, fc * 128:(fc + 1) * 128], identity=cst["ident_f"][:]),
                                reads=[bxt[sl], cstb["ident_f"]], writes=[PBb[pbi]])
                        if fc % 2 == 0:
                            S.op("act", lambda e, fc=fc, tg=tg, pbi=pbi: e.activation(
                                out=hT[:, fc, tg * 512:(tg + 1) * 512], in_=PB[pbi][:, :], func=AF.Identity,
                                bias=modF[:, fc:fc + 1], scale=modF[:, 8 + fc:9 + fc]), reads=[PBb[pbi], bmodF], writes=[bhT[tg]])
                        else:
                            S.op("dve", lambda e, fc=fc, tg=tg, pbi=pbi: e.tensor_scalar(
                                out=hT[:, fc, tg * 512:(tg + 1) * 512], in0=PB[pbi][:, :], scalar1=modF[:, 8 + fc:9 + fc],
                                scalar2=modF[:, fc:fc + 1], op0=ALU.mult, op1=ALU.add), reads=[PBb[pbi], bmodF], writes=[bhT[tg]])
                do_piece(4 + tg)
            S.op("dve", lambda e: e.tensor_scalar_add(out=s2b[:], in0=s2b[:], scalar1=1.0), reads=[bs2b], writes=[bs2b])
            S.barrier()
        if stop_after == 0:
            dump(modF[:], bmodF, 16)
            dump(g1b[:], bg1b, 1024, col0=16)
            dump(s2b[:], bs2b, 1024, col0=16 + 1024)
        if stop_after == 1:
            for fc in range(8):
                pass
            tmpd = sb("tmpd", [128, 2048], F32); btmpd = S.buf("tmpd")
            S.op("dve", lambda e: e.tensor_copy(out=tmpd[:, 0:1024].rearrange("p (f t) -> p f t", f=8), in_=hT[:, :, 3072:3200]), reads=bhT, writes=[btmpd])
            S.op("dve", lambda e: e.tensor_copy(out=tmpd[:, 1024:2048], in_=hT[:, 7, 3072:4096]), reads=bhT, writes=[btmpd])
            dump(tmpd[:], btmpd, 2048)

        cumT = sbA("cumT", [128, NT, 8], F32); bcumT = S.buf("cumT")
        tpre = sbA("tpre", [128, NT + 1, 8], F32); btpre = S.buf("tpre")
        if stop_after >= 2:
            with ExitStack() as st2:
                sb2 = lambda n, s, d: st2.enter_context(nc.sbuf_tensor(n, s, d))
                wff = sb2("wff", [128, 8, 8], BF16); bwff = S.buf("wff")
                bfb = sb2("bfb", [128, 8], F32); bbfb = S.buf("bfb")
                zz = sb2("zz", [128, NT, 8], F32); bzz = S.buf("zz")
                tot = sb2("tot", [128, NT, 8], F32); btot = S.buf("tot")
                one1 = sb2("one1", [128, 1], F32); bone1 = S.buf("one1")
                wff_f = sb2("wff_f", [128, 8, 8], F32); bwff_f = S.buf("wff_f")
                S.dma("sp", lambda e: e.dma_start(out=wff_f[:], in_=win_v[:, :, 2048:2056]), writes=[bwff_f])
                S.op("dve", lambda e: e.tensor_copy(out=wff[:], in_=wff_f[:]), reads=[bwff_f], writes=[bwff])
                S.dma("sp", lambda e: e.dma_start(out=bfb[:], in_=bf_d.broadcast_to([128, 8])), writes=[bbfb])
                S.op("pool", lambda e: e.memset(one1[:], 1.0), writes=[bone1])
                for tt in range(NT):
                    for kc in range(8):
                        S.op("pe", lambda e, tt=tt, kc=kc: e.matmul(PB[4][:, tt * 8:(tt + 1) * 8], lhsT=hT[:, kc, tt * 128:(tt + 1) * 128],
                                                                    rhs=wff[:, kc, :], start=(kc == 0), stop=(kc == 7)),
                             reads=[bhT[tt // 4], bwff], writes=[PBb[4]])
                S.op("dve", lambda e: e.tensor_tensor(out=zz[:], in0=PB[4][:, 0:256].rearrange("p (t h) -> p t h", h=8),
                                                      in1=bfb[:].unsqueeze(1).broadcast_to([128, NT, 8]), op=ALU.add),
                     reads=[PBb[4], bbfb], writes=[bzz])
                if stop_after == 2 and debug:
                    dump(zz[:].rearrange("p t h -> p (t h)"), bzz, 256, col0=1024)
                S.op("act", lambda e: e.activation(out=zz[:], in_=zz[:], func=AF.Exp, scale=-1.0), reads=[bzz], writes=[bzz])
                S.op("act", lambda e: e.activation(out=zz[:], in_=zz[:], func=AF.Ln, bias=one1[:, 0:1], scale=1.0), reads=[bzz, bone1], writes=[bzz])
                zf = zz[:].rearrange("p t h -> p (t h)")
                S.op("pe", lambda e: e.matmul(PB[5][:, 0:256], lhsT=cst["negU"][:], rhs=zf, start=True, stop=True), reads=[bzz, cstb["negU"]], writes=[PBb[5]])
                S.op("pe", lambda e: e.matmul(PB[6][:, 0:256], lhsT=cst["negOnes"][:], rhs=zf, start=True, stop=True), reads=[bzz, cstb["negOnes"]], writes=[PBb[6]])
                S.op("dve", lambda e: e.tensor_copy(out=tot[:].rearrange("p t h -> p (t h)"), in_=PB[6][:, 0:256]), reads=[PBb[6]], writes=[btot])
                S.op("pool", lambda e: e.memset(tpre[:, 0, :], 0.0), writes=[btpre])
                for i in range(1, NT + 1):
                    S.op("dve", lambda e, i=i: e.tensor_tensor(out=tpre[:, i, :], in0=tpre[:, i - 1, :], in1=tot[:, i - 1, :], op=ALU.add),
                         reads=[btot, btpre], writes=[btpre])
                S.op("dve", lambda e: e.tensor_tensor(out=cumT[:], in0=PB[5][:, 0:256].rearrange("p (t h) -> p t h", h=8), in1=tpre[:, 0:NT, :], op=ALU.add),
                     reads=[PBb[5], btpre], writes=[bcumT])
                S.barrier()
        if stop_after == 2:
            dump(cumT[:].rearrange("p t h -> p (t h)"), bcumT, 256)
            dump(tpre[:].rearrange("p t h -> p (t h)"), btpre, 264, col0=256)

        cosT = sbA("cosT", [128, S_LEN], BF16); bcos = S.buf("cosT")
        sinT = sbA("sinT", [128, S_LEN], BF16); bsin = S.buf("sinT")
        if stop_after >= 4:
            with ExitStack() as str_:
                sbr = lambda n, s, d: str_.enter_context(nc.sbuf_tensor(n, s, d))
                posi = sbr("posi", [128, S_LEN], I32); bposi = S.buf("posi")
                A_ = sbr("angA", [128, S_LEN], F32); bA = S.buf("angA")
                T_ = sbr("angT", [128, S_LEN], F32); bT = S.buf("angT")
                TWO_PI = 2.0 * math.pi
                C1 = 6.28125
                C2 = TWO_PI - C1
                S.dma("sp", lambda e: e.dma_start(out=posi[:], in_=pos_d.broadcast_to([128, S_LEN])), writes=[bposi])
                S.op("dve", lambda e: e.tensor_copy(out=T_[:], in_=posi[:]), reads=[bposi], writes=[bT])
                S.op("dve", lambda e: e.tensor_scalar(out=A_[:], in0=T_[:], scalar1=cst["invf"][:, 0:1], scalar2=None, op0=ALU.mult), reads=[bT, cstb["invf"]], writes=[bA])

                def reduce_and_sin(dst, bdst, shift):
                    S.op("dve", lambda e: e.tensor_scalar(out=T_[:], in0=A_[:], scalar1=1.0 / TWO_PI, scalar2=shift / TWO_PI, op0=ALU.mult, op1=ALU.add), reads=[bA], writes=[bT])
                    S.op("dve", lambda e: e.tensor_copy(out=posi[:], in_=T_[:]), reads=[bT], writes=[bposi])
                    S.op("dve", lambda e: e.tensor_copy(out=T_[:], in_=posi[:]), reads=[bposi], writes=[bT])
                    U_ = posi[:].bitcast(F32)
                    S.op("dve", lambda e: e.scalar_tensor_tensor(out=U_, in0=T_[:], scalar=-C1, in1=A_[:], op0=ALU.mult, op1=ALU.add), reads=[bT, bA], writes=[bposi])
                    S.op("dve", lambda e: e.scalar_tensor_tensor(out=U_, in0=T_[:], scalar=-C2, in1=U_, op0=ALU.mult, op1=ALU.add), reads=[bT, bposi], writes=[bposi])
                    if shift != 0.0:
                        S.op("dve", lambda e: e.tensor_scalar_add(out=U_, in0=U_, scalar1=shift), reads=[bposi], writes=[bposi])
                    S.op("dve", lambda e: e.tensor_single_scalar(out=T_[:], in_=U_, scalar=math.pi, op=ALU.is_gt), reads=[bposi], writes=[bT])
                    S.op("dve", lambda e: e.scalar_tensor_tensor(out=U_, in0=T_[:], scalar=-TWO_PI, in1=U_, op0=ALU.mult, op1=ALU.add), reads=[bT, bposi], writes=[bposi])
                    S.op("dve", lambda e: e.tensor_single_scalar(out=T_[:], in_=U_, scalar=-math.pi, op=ALU.is_lt), reads=[bposi], writes=[bT])
                    S.op("dve", lambda e: e.scalar_tensor_tensor(out=U_, in0=T_[:], scalar=TWO_PI, in1=U_, op0=ALU.mult, op1=ALU.add), reads=[bT, bposi], writes=[bposi])
                    S.op("act", lambda e: e.activation(out=dst[:], in_=U_, func=AF.Sin), reads=[bposi], writes=[bdst])

                reduce_and_sin(sinT, bsin, 0.0)
                reduce_and_sin(cosT, bcos, math.pi / 2.0)
                S.barrier()
        if stop_after == 4 and debug and False:
            pass

        if stop_after >= 3:
            with ExitStack() as st3:
                sb3 = lambda n, s, d: st3.enter_context(nc.sbuf_tensor(n, s, d))
                wq = sb3("wq", [128, 8, 128], BF16); bwq = S.buf("wq")
                wk = sb3("wk", [128, 8, 128], BF16); bwk = S.buf("wk")
                wv = sb3("wv", [128, 8, 128], BF16); bwv = S.buf("wv")
                wg = sb3("wg", [128, 8, 128], BF16); bwg = S.buf("wg")
                QZ = [sb3("QZ%d" % i, [128, S_LEN], BF16) for i in range(2)]
                bQT = S.buf("QZ")
                bQzero = S.buf("QZzero")
                S.op("pool", lambda e: e.memset(QZ[0][64:128, :], 0.0), writes=[bQzero])
                S.op("pool", lambda e: e.memset(QZ[1][0:64, :], 0.0), writes=[bQT])
                bzx = S.buf("zxe")
                for zi in range(TOTSLOT // 256):
                    S.dma("sp", lambda e, zi=zi: e.dma_start(out=Xe_d[zi * 256:(zi + 1) * 256, :].rearrange("(p j) f -> p (j f)", p=64), in_=QZ[0][64:128, :]),
                          reads=[bQzero], writes=[], semb=bzx)
                KT = sb3("KT", [128, S_LEN], BF16); bKT = S.buf("KT")
                GT = sb3("GT", [128, S_LEN], BF16); bGT = S.buf("GT")
                Va = sb3("Va", [128, NT, 2, 65], BF16); bVa = S.buf("Va")
                biasT = sb3("biasT", [128, 16, NT], F32); bbias = S.buf("biasT")
                Vs = sb3("Vs", [128, 8, 65], BF16); bVs = [S.buf("Vs%d" % i) for i in range(8)]
                PT = [sb3("PT%d" % i, [128, 512], BF16) for i in range(4)]
                bPT = [S.buf("PT%d" % i) for i in range(4)]
                osb = [sb3("osb%d" % i, [65, 256], F32) for i in range(2)]
                bosb = [S.buf("osb%d" % i) for i in range(2)]
                rrow = osb
                brrow = bosb
                stage0 = sb3("stage0", [64, S_LEN], BF16)
                stage = [stage0, stage0]
                bstage0 = S.buf("stage0")
                bstage = [bstage0, bstage0]
                bmT = S.buf("mTdram"); bGd = S.buf("Gdram"); bvd = S.buf("vddram")
                S.op("pool", lambda e: e.memset(Va[:, :, :, 64:65], 1.0), writes=[bVa])

                def load_w(tile, b, col0, q):
                    S.dma("pool", lambda e: e.dma_start(out=tile[:], in_=win_v[:, :, col0:col0 + 128]), writes=[b])

                def proj_fm(wt, bw, evac):
                    for tg in range(8):
                        pbi = tg % 2
                        for kc in range(8):
                            S.op("pe", lambda e, kc=kc, tg=tg, pbi=pbi: e.matmul(PB[pbi][:, :], lhsT=wt[:, kc, :], rhs=hT[:, kc, tg * 512:(tg + 1) * 512],
                                                                              start=(kc == 0), stop=(kc == 7)), reads=[bw, bhT[tg]], writes=[PBb[pbi]])
                        evac(tg, pbi)

                def proj_v(wt, bw):
                    for t4 in range(8):
                        pbi = 2 + (t4 % 2)
                        for j in range(4):
                            tt = t4 * 4 + j
                            for kc in range(8):
                                S.op("pe", lambda e, kc=kc, tt=tt, j=j, pbi=pbi: e.matmul(PB[pbi][:, j * 128:(j + 1) * 128], lhsT=hT[:, kc, tt * 128:(tt + 1) * 128],
                                                                                      rhs=wt[:, kc, :], start=(kc == 0), stop=(kc == 7)),
                                     reads=[bw, bhT[tt // 4]], writes=[PBb[pbi]])
                        S.op("dve", lambda e, t4=t4, pbi=pbi: e.tensor_copy(out=Va[:, t4 * 4:(t4 + 1) * 4, :, 0:64],
                                                                          in_=PB[pbi][:, :].rearrange("p (t h d) -> p t h d", t=4, h=2)),
                             reads=[PBb[pbi]], writes=[bVa])

                def normalize(src_ps_or_sb, bsrc, ncols, dst_ap, bdst, k):
                    o = osb[k]; r = rrow[k]
                    S.op("dve", lambda e: e.tensor_copy(out=o[:, 0:ncols], in_=src_ps_or_sb), reads=[bsrc], writes=[bosb[k]])
                    S.op("dve", lambda e: e.reciprocal(out=r[64:65, 0:ncols], in_=o[64:65, 0:ncols]), reads=[bosb[k]], writes=[brrow[k]])

                    def part2():
                        S.op("pe", lambda e: e.matmul(PB[7][0:64, 0:ncols], lhsT=cst["ones_f"][64:65, 0:64], rhs=r[64:65, 0:ncols], start=True, stop=True),
                             reads=[brrow[k], cstb["ones_f"]], writes=[PBb[7]])
                        S.op("dve", lambda e: e.tensor_tensor(out=dst_ap, in0=o[0:64, 0:ncols], in1=PB[7][0:64, 0:ncols], op=ALU.mult),
                             reads=[bosb[k], PBb[7]], writes=[bdst])
                    return part2

                n_fox = 0 if (stop_after == 4 and debug) else 4
                for pj in range(n_fox):
                    load_w(wq, bwq, 128 * pj, "sp")
                    load_w(wk, bwk, 512 + 128 * pj, "pool")
                    load_w(wv, bwv, 1024 + 128 * pj, "sp")
                    load_w(wg, bwg, 1536 + 128 * pj, "pool")
                    def q_evac(tg, pbi):
                        S.op("act", lambda e: e.copy(out=QZ[0][0:64, tg * 512:(tg + 1) * 512], in_=PB[pbi][0:64, :]), reads=[PBb[pbi]], writes=[bQT])
                        S.op("dve", lambda e: e.tensor_copy(out=QZ[1][64:128, tg * 512:(tg + 1) * 512], in_=PB[pbi][64:128, :]), reads=[PBb[pbi]], writes=[bQT])
                    proj_fm(wq, bwq, q_evac)
                    proj_fm(wk, bwk, lambda tg, pbi: S.op("dve", lambda e: e.tensor_copy(out=KT[:, tg * 512:(tg + 1) * 512], in_=PB[pbi][:, :]), reads=[PBb[pbi]], writes=[bKT]))
                    proj_fm(wg, bwg, lambda tg, pbi: S.op("act", lambda e: e.activation(out=GT[:, tg * 512:(tg + 1) * 512], in_=PB[pbi][:, :], func=AF.Sigmoid), reads=[PBb[pbi]], writes=[bGT]))
                    S.dma("sp", lambda e, pj=pj: e.dma_start(out=G_d[pj], in_=GT[:]), reads=[bGT], writes=[bGd])
                    proj_v(wv, bwv)
                    for hh in range(2):
                        h = 2 * pj + hh
                        d0 = 64 * hh
                        stg = stage[hh]; bstg = bstage[hh]
                        S.op("pool", lambda e: e.memset(biasT[:, :, :], 0.0), writes=[bbias])
                        for g in range(16):
                            S.op("dve", lambda e, g=g, h=h: e.tensor_scalar(out=biasT[:, g, 0:2 * g + 2], in0=cumT[:, 0:2 * g + 2, h], scalar1=-1.0,
                                                                           scalar2=tpre[:, 2 * g, h:h + 1], op0=ALU.mult, op1=ALU.add),
                                 reads=[bcumT, btpre], writes=[bbias])
                        S.op("act", lambda e: e.activation(out=biasT[:, :, :], in_=biasT[:, :, :], func=AF.Exp), reads=[bbias], writes=[bbias])
                        iters = []
                        for g in range(16):
                            nkb = 2 * g + 2
                            for kb in range(0, nkb, 2):
                                iters.append((g, kb, nkb))
                        DEPTH = 3

                        def fox_front(idx, iters=iters, h=h, hh=hh, d0=d0):
                            g, kb, nkb = iters[idx]
                            diag = (kb == 2 * g)
                            n2 = 128 if diag else 256
                            q2 = 128 if diag else 0
                            si = idx % 4
                            vk = (2 * idx) % 8
                            S.op("pe", lambda e: e.matmul(
                                PB[si][:, 0:256], lhsT=KT[:, kb * 128:(kb + 1) * 128], rhs=QZ[hh][:, 256 * g:256 * g + 256],
                                start=True, stop=True), reads=[bKT, bQT, bQzero], writes=[PBb[si]])
                            S.op("pe", lambda e: e.matmul(
                                PB[si][:, 256:256 + n2], lhsT=KT[:, (kb + 1) * 128:(kb + 2) * 128], rhs=QZ[hh][:, 256 * g + q2:256 * g + 256],
                                start=True, stop=True), reads=[bKT, bQT, bQzero], writes=[PBb[si]])
                            S.op("act", lambda e: e.activation(out=PT[si][:, 0:256 + n2], in_=PB[si][:, 0:256 + n2], func=AF.Exp, scale=0.125),
                                 reads=[PBb[si]], writes=[bPT[si]])
                            if diag:
                                S.op("pool", lambda e: e.tensor_tensor(out=PT[si][:, 0:512].rearrange("p (a c) -> p a c", a=2)[:, :, 0:128],
                                                                       in0=PT[si][:, 0:512].rearrange("p (a c) -> p a c", a=2)[:, :, 0:128],
                                                                       in1=cst["mask4"][:, 128:256].unsqueeze(1).broadcast_to([128, 2, 128]), op=ALU.mult),
                                     reads=[bPT[si], cstb["mask4"]], writes=[bPT[si]])
                            S.op("dve", lambda e: e.tensor_tensor(out=Vs[:, vk:vk + 2, :], in0=Va[:, kb:kb + 2, hh, :],
                                                                  in1=biasT[:, g, kb:kb + 2].unsqueeze(2).broadcast_to([128, 2, 65]), op=ALU.mult),
                                 reads=[bVa, bbias], writes=[bVs[vk], bVs[vk + 1]])

                        def fox_back(idx, iters=iters, h=h, hh=hh, d0=d0, stg=stg, bstg=bstg):
                            g, kb, nkb = iters[idx]
                            diag = (kb == 2 * g)
                            n2 = 128 if diag else 256
                            q2 = 128 if diag else 0
                            si = idx % 4
                            vk = (2 * idx) % 8
                            po = 4 + (g % 2)
                            S.op("pe", lambda e: e.matmul(
                                PB[po][0:65, 0:256], lhsT=Vs[:, vk, :], rhs=PT[si][:, 0:256], start=(kb == 0), stop=False),
                                reads=[bVs[vk], bPT[si]], writes=[PBb[po]])
                            S.op("pe", lambda e: e.matmul(
                                PB[po][0:65, q2:256], lhsT=Vs[:, vk + 1, :], rhs=PT[si][:, 256:256 + n2], start=False, stop=(kb + 2 == nkb)),
                                reads=[bVs[vk + 1], bPT[si]], writes=[PBb[po]])
                            if kb + 2 == nkb:
                                pending.append([4, normalize(PB[po][0:65, 0:256], PBb[po], 256, stg[:, 256 * g:256 * (g + 1)], bstg, g % 2)])

                        pending = []
                        for idx in range(len(iters) + DEPTH):
                            if idx < len(iters):
                                fox_front(idx)
                            if idx - DEPTH >= 0:
                                fox_back(idx - DEPTH)
                            for p_ in pending:
                                p_[0] -= 1
                            while pending and pending[0][0] <= 0:
                                pending.pop(0)[1]()
                        while pending:
                            pending.pop(0)[1]()
                        S.dma("sp", lambda e, pj=pj, d0=d0, stg=stg: e.dma_start(out=mT_d[pj, d0:d0 + 64, :], in_=stg[:]), reads=[bstg], writes=[bmT])

                if stop_after >= 4:
                    Vg = {4: sb3("Vg4", [128, NT, 130], BF16), 16: sb3("Vg16", [128, NT, 130], BF16)}
                    bVg = {4: S.buf("Vg4"), 16: S.buf("Vg16")}
                    acc = sb3("acc", [65, S_LEN], F32); bacc = S.buf("acc")
                    qa = [sb3("qa%d" % i, [128, 512], BF16) for i in range(2)]
                    bqa = [S.buf("qa%d" % i) for i in range(2)]
                    S.barrier()
                    GTf = GT[:].bitcast(F32)
                    t1 = [GTf[:, i * 512:(i + 1) * 512] for i in range(2)]
                    bt1 = [S.buf("t1_%d" % i) for i in range(2)]
                    t2 = [GTf[:, (2 + i) * 512:(3 + i) * 512] for i in range(2)]
                    bt2 = [S.buf("t2_%d" % i) for i in range(2)]
                    Vaf = Va[:].rearrange("p t h c -> p t (h c)")

                    def rope_evac(dstT, bdstT):
                        def ev(tg, pbi):
                            k = tg % 2
                            cols = slice(tg * 512, (tg + 1) * 512)
                            S.op("act", lambda e: e.copy(out=qa[k][:], in_=PB[pbi][:, :]), reads=[PBb[pbi]], writes=[bqa[k]])
                            S.op("pe", lambda e: e.matmul(PB[2 + k][:, :], lhsT=cst["rotm"][:], rhs=qa[k][:], start=True, stop=True),
                                 reads=[bqa[k], cstb["rotm"]], writes=[PBb[2 + k]])
                            S.op("dve", lambda e: e.tensor_tensor(out=t1[k], in0=qa[k][:], in1=cosT[:, cols], op=ALU.mult), reads=[bqa[k], bcos], writes=[bt1[k]])
                            S.op("dve", lambda e: e.tensor_tensor(out=t2[k], in0=PB[2 + k][:, :], in1=sinT[:, cols], op=ALU.mult), reads=[PBb[2 + k], bsin], writes=[bt2[k]])
                            if dstT is None:
                                S.op("pool", lambda e: e.tensor_tensor(out=QZ[0][0:64, cols], in0=t1[k][0:64, :], in1=t2[k][0:64, :], op=ALU.add), reads=[bt1[k], bt2[k]], writes=[bdstT])
                                S.op("pool", lambda e: e.tensor_tensor(out=QZ[1][64:128, cols], in0=t1[k][64:128, :], in1=t2[k][64:128, :], op=ALU.add), reads=[bt1[k], bt2[k]], writes=[bdstT])
                            else:
                                S.op("pool", lambda e: e.tensor_tensor(out=dstT[:, cols], in0=t1[k], in1=t2[k], op=ALU.add), reads=[bt1[k], bt2[k]], writes=[bdstT])
                        return ev

                    dil_deferred = []
                    for pj in range(4):
                        load_w(wq, bwq, 2056 + 128 * pj, "pool")
                        load_w(wk, bwk, 2568 + 128 * pj, "pool")
                        load_w(wv, bwv, 3080 + 128 * pj, "pool")
                        proj_fm(wq, bwq, rope_evac(None, bQT))
                        proj_fm(wk, bwk, rope_evac(KT, bKT))
                        proj_v(wv, bwv)
                        S.dma("sp", lambda e, pj=pj: e.dma_start(out=vd_d[pj].rearrange("(t p) h c -> p t (h c)", p=128), in_=Vaf), reads=[bVa], writes=[bvd])
                        for dl in (4, 16):
                            nb = NT // dl
                            src = vd_d[pj].rearrange("(n p r) h c -> p r n (h c)", p=128, r=dl)
                            for r in range(dl):
                                S.dma("sp", lambda e, dl=dl, r=r, nb=nb, src=src: e.dma_start(out=Vg[dl][:, r * nb:(r + 1) * nb, :], in_=src[:, r, :, :]),
                                      reads=[bvd], writes=[bVg[dl]])
                        for hh in range(2):
                            d0 = 64 * hh
                            stg = stage[hh]; bstg = bstage[hh]
                            tiles = []
                            bank_ctr = 0
                            for dl in (1, 4, 16):
                                nb = NT // dl
                                blocks = [(r, n) for r in range(dl) for n in range(nb)]
                                for b0 in range(0, len(blocks), 4):
                                    po = 4 + (bank_ctr % 2)
                                    bank_ctr += 1
                                    runs = []
                                    for bi in range(4):
                                        r, n = blocks[b0 + bi]
                                        if runs and runs[-1][0] == r:
                                            runs[-1][2] += 1
                                        else:
                                            runs.append([r, n, 1, bi])
                                    tiles.append((dl, blocks[b0:b0 + 2], po, 0, None))
                                    tiles.append((dl, blocks[b0 + 2:b0 + 4], po, 256, runs))

                            def vblk(dl, r, n, hh=hh):
                                if dl == 1:
                                    return Va[:, n, hh, :], bVa
                                nb = NT // dl
                                return Vg[dl][:, r * nb + n, hh * 65:(hh + 1) * 65], bVg[dl]

                            def qk(T, dl, r, n, hh=hh):
                                T = KT if T is KT else QZ[hh]
                                st_ = r + dl * 128 * n
                                return T[:, st_:st_ + dl * 127 + 1:dl] if dl > 1 else T[:, st_:st_ + 128]

                            def dil_front(idx, tiles=tiles, qk=qk):
                                dl, blks, po, c0, runs = tiles[idx]
                                si = idx % 4
                                for bl, (r, n) in enumerate(blks):
                                    if n > 0:
                                        S.op("pe", lambda e, r=r, n=n, bl=bl: e.matmul(
                                            PB[si][:, bl * 256:bl * 256 + 128], lhsT=qk(KT, dl, r, n - 1), rhs=qk(None, dl, r, n), start=True, stop=True),
                                            reads=[bKT, bQT, bQzero], writes=[PBb[si]])
                                    S.op("pe", lambda e, r=r, n=n, bl=bl: e.matmul(
                                        PB[si][:, bl * 256 + 128:bl * 256 + 256], lhsT=qk(KT, dl, r, n), rhs=qk(None, dl, r, n), start=True, stop=True),
                                        reads=[bKT, bQT, bQzero], writes=[PBb[si]])
                                S.op("act", lambda e: e.activation(out=PT[si][:, :], in_=PB[si][:, :], func=AF.Exp, scale=0.125),
                                     reads=[PBb[si]], writes=[bPT[si]])
                                meng = "dve" if (idx % 5) in (1, 3) else "pool"
                                S.op(meng, lambda e: e.tensor_tensor(out=PT[si][:, :], in0=PT[si][:, :], in1=cst["mask4"][:, :], op=ALU.mult),
                                     reads=[bPT[si], cstb["mask4"]], writes=[bPT[si]])

                            def dil_back(idx, tiles=tiles, vblk=vblk):
                                dl, blks, po, c0, runs = tiles[idx]
                                si = idx % 4
                                for bl, (r, n) in enumerate(blks):
                                    bc = c0 + bl * 128
                                    if n > 0:
                                        va, bva = vblk(dl, r, n - 1)
                                        S.op("pe", lambda e, va=va, bl=bl, bc=bc: e.matmul(
                                            PB[po][0:65, bc:bc + 128], lhsT=va, rhs=PT[si][:, bl * 256:bl * 256 + 128], start=True, stop=False),
                                            reads=[bva, bPT[si]], writes=[PBb[po]])
                                    va, bva = vblk(dl, r, n)
                                    S.op("pe", lambda e, va=va, bl=bl, bc=bc, n=n: e.matmul(
                                        PB[po][0:65, bc:bc + 128], lhsT=va, rhs=PT[si][:, bl * 256 + 128:bl * 256 + 256], start=(n == 0), stop=True),
                                        reads=[bva, bPT[si]], writes=[PBb[po]])
                                if runs is not None:
                                    for (r, n, cnt_, bi) in runs:
                                        st_ = r + dl * 128 * n
                                        ncol = cnt_ * 128
                                        dst = acc[0:65, st_:st_ + dl * (ncol - 1) + 1:dl] if dl > 1 else acc[0:65, st_:st_ + ncol]
                                        srcp = PB[po][0:65, bi * 128:bi * 128 + ncol]
                                        if dl == 1:
                                            S.op("act", lambda e, dst=dst, srcp=srcp: e.copy(out=dst, in_=srcp), reads=[PBb[po]], writes=[bacc])
                                        else:
                                            S.op("dve", lambda e, dst=dst, srcp=srcp: e.tensor_tensor(out=dst, in0=srcp, in1=dst, op=ALU.add), reads=[PBb[po], bacc], writes=[bacc])

                            DEPTH = 3
                            for idx in range(len(tiles) + DEPTH):
                                if idx < len(tiles):
                                    dil_front(idx)
                                if idx == DEPTH - 1 and dil_deferred:
                                    dil_deferred.pop(0)()
                                if idx - DEPTH >= 0:
                                    dil_back(idx - DEPTH)

                            def norm_job(pj=pj, d0=d0, stg=stg, bstg=bstg):
                                S.op("act", lambda e: e.activation(out=acc[64:65, :], in_=acc[64:65, :], func=AF.Ln), reads=[bacc], writes=[bacc])
                                S.op("act", lambda e: e.activation(out=acc[64:65, :], in_=acc[64:65, :], func=AF.Exp, scale=-1.0), reads=[bacc], writes=[bacc])
                                for cg in range(8):
                                    cols = slice(cg * 512, (cg + 1) * 512)
                                    S.op("pe", lambda e, cols=cols: e.matmul(PB[7][0:64, :], lhsT=cst["ones_f"][64:65, 0:64], rhs=acc[64:65, cols], start=True, stop=True),
                                         reads=[bacc, cstb["ones_f"]], writes=[PBb[7]])
                                    S.op("dve", lambda e, cols=cols: e.tensor_tensor(out=stg[:, cols], in0=acc[0:64, cols], in1=PB[7][0:64, :], op=ALU.mult),
                                         reads=[bacc, PBb[7]], writes=[bstg])
                                S.dma("sp", lambda e: e.dma_start(out=mT_d[4 + pj, d0:d0 + 64, :], in_=stg[:]), reads=[bstg], writes=[bmT])
                            dil_deferred.append(norm_job)
                    while dil_deferred:
                        dil_deferred.pop(0)()

                if stop_after == 4:
                    S.op("dve", lambda e: e.tensor_copy(out=acc[0:64, :], in_=stage[0][:]), reads=[bstage[0]], writes=[bacc])
                    dump(acc[0:64, :], bacc, S_LEN, part=64)
                    S.op("dve", lambda e: e.tensor_copy(out=acc[0:64, :], in_=stage[1][:]), reads=[bstage[1], bdbg], writes=[bacc])
                    dump(acc[0:64, :], bacc, S_LEN, part=64, col0=S_LEN)
                if stop_after == 3:
                    tmpd = sb3("tmpd3", [64, S_LEN], F32); btmpd = S.buf("tmpd3")
                    S.op("dve", lambda e: e.tensor_copy(out=tmpd[:], in_=stage[0][:]), reads=[bstage[0]], writes=[btmpd])
                    dump(tmpd[:], btmpd, S_LEN, part=64)
                    S.op("dve", lambda e: e.tensor_copy(out=tmpd[:], in_=stage[1][:]), reads=[bstage[1], bdbg], writes=[btmpd])
                    dump(tmpd[:], btmpd, S_LEN, part=64, col0=S_LEN)

        S.barrier()
        stA.close()

        RL = sb("RL", [128, NT, 36], F32); bRL = S.buf("RL")
        idxW = sb("idxW", [128, NEXP], I32); bidxW = S.buf("idxW")
        epsT = sb("epsT", [128, 1], F32); bepsT = S.buf("epsT")
        S.op("pool", lambda e: e.memset(epsT[:], LN_EPS), writes=[bepsT])
        slot_i = [sb("slot%d" % k, [128, NT], I32) for k in range(2)]
        bslot = [S.buf("slot%d" % k) for k in range(2)]
        wsel = [sb("wsel%d" % k, [128, NT], F32) for k in range(2)]
        bwsel = [S.buf("wsel%d" % k) for k in range(2)]
        bX1 = S.buf("X1dram"); bH2 = S.buf("H2dram"); bXe = S.buf("Xedram"); bYs = S.buf("Ysdram")

        def layer_norm_tile(src, bsrc, dst, bdst, gam, bgam, bet, bbet, st6, bst6, mv, bmv):
            for hf in range(2):
                S.op("dve", lambda e, hf=hf: e.bn_stats(out=st6[:, hf, :], in_=src[:, hf * 512:(hf + 1) * 512]), reads=[bsrc], writes=[bst6])
            S.op("dve", lambda e: e.bn_aggr(out=mv[:, 0:2], in_=st6[:].rearrange("p a b -> p (a b)")), reads=[bst6], writes=[bmv])
            S.op("act", lambda e: e.activation(out=mv[:, 2:3], in_=mv[:, 1:2], func=AF.Sqrt, bias=epsT[:, 0:1], scale=1.0), reads=[bmv, bepsT], writes=[bmv])
            S.op("dve", lambda e: e.reciprocal(out=mv[:, 2:3], in_=mv[:, 2:3]), reads=[bmv], writes=[bmv])
            S.op("dve", lambda e: e.scalar_tensor_tensor(out=mv[:, 3:4], in0=mv[:, 0:1], scalar=-1.0, in1=mv[:, 2:3], op0=ALU.mult, op1=ALU.mult), reads=[bmv], writes=[bmv])
            S.op("act", lambda e: e.activation(out=dst[:], in_=src[:], func=AF.Identity, bias=mv[:, 3:4], scale=mv[:, 2:3]), reads=[bsrc, bmv], writes=[bdst])
            S.op("pool", lambda e: e.tensor_tensor(out=dst[:], in0=dst[:], in1=gam[:], op=ALU.mult), reads=[bdst, bgam], writes=[bdst])
            S.op("dve", lambda e: e.tensor_tensor(out=dst[:], in0=dst[:], in1=bet[:], op=ALU.add), reads=[bdst, bbet], writes=[bdst])

        def run_pipeline(stages, n):
            for step in range(n + len(stages) - 1):
                for si in reversed(range(len(stages))):
                    t = step - si
                    if 0 <= t < n:
                        stages[si](t)

        if stop_after >= 5:
            with ExitStack() as st5:
                sb5 = lambda n, s_, d: st5.enter_context(nc.sbuf_tensor(n, s_, d))
                wo = sb5("wo", [128, 8, D], BF16); bwo = S.buf("wo")
                ln1g = sb5("ln1g", [128, D], F32); bln1g = S.buf("ln1g")
                ln1b = sb5("ln1b", [128, D], F32); bln1b = S.buf("ln1b")
                wr = sb5("wr", [128, 8, 36], F32); bwr = S.buf("wr")
                brb = sb5("brb", [128, 36], F32); bbrb = S.buf("brb")

                def ring(name, shape, dt, n):
                    return [sb5("%s_%d" % (name, k), shape, dt) for k in range(n)], [S.buf("%s_%d" % (name, k)) for k in range(n)]
                mTt, bmTt = ring("mTt", [128, 8, 128], BF16, 4)
                Gt, bGt = ring("Gt", [128, 4, 128], BF16, 3)
                xt5, bxt5 = ring("xt5", [128, D], F32, 5)
                t25, bt25 = ring("t25", [128, D], F32, 5)
                x15, bx15 = ring("x15", [128, D], F32, 5)
                h25, bh25 = ring("h25", [128, D], F32, 4)
                h2b, bh2b = ring("h2b", [128, D], BF16, 3)
                h2T, bh2T = ring("h2T", [128, 8, 128], F32, 3)
                st6, bst6 = ring("st6", [128, 2, 6], F32, 6)
                mv, bmv = ring("mv", [128, 4], F32, 6)
                wof = sb5("wof", [128, 8, D], F32); bwof = S.buf("wof")
                S.dma("sp", lambda e: e.dma_start(out=wof[:], in_=wout_d.rearrange("(kc p) c -> p kc c", p=128)), writes=[bwof])
                for kc in range(8):
                    S.op("pool" if kc % 2 else "dve", lambda e, kc=kc: e.tensor_tensor(out=wo[:, kc, :], in0=wof[:, kc, :], in1=g1b[:], op=ALU.mult), reads=[bwof, bg1b], writes=[bwo])
                S.dma("sp", lambda e: e.dma_start(out=ln1g[:], in_=ln1g_d.broadcast_to([128, D])), writes=[bln1g])
                S.dma("sp", lambda e: e.dma_start(out=ln1b[:], in_=ln1b_d.broadcast_to([128, D])), writes=[bln1b])
                S.dma("sp", lambda e: e.dma_start(out=wr[:, :, 0:4], in_=wrg_d.rearrange("(kc p) c -> p kc c", p=128)), writes=[bwr])
                S.dma("sp", lambda e: e.dma_start(out=wr[:, :, 4:36], in_=wre_d.rearrange("(kc p) c -> p kc c", p=128)), writes=[bwr])
                S.dma("sp", lambda e: e.dma_start(out=brb[:, 0:4], in_=brg_d.broadcast_to([128, 4])), writes=[bbrb])
                S.dma("sp", lambda e: e.dma_start(out=brb[:, 4:36], in_=bre_d.broadcast_to([128, 32])), writes=[bbrb])

                def R(lst, tt):
                    return lst[tt % len(lst)]

                def s_load(tt):
                    cols = slice(tt * 128, (tt + 1) * 128)
                    S.dma("sp", lambda e: e.dma_start(out=R(mTt, tt)[:], in_=mT_d[:, :, cols].rearrange("c p t -> p c t")), writes=[R(bmTt, tt)])
                    S.dma("sp", lambda e: e.dma_start(out=R(Gt, tt)[:], in_=G_d[:, :, cols].rearrange("c p t -> p c t")), writes=[R(bGt, tt)])
                    S.dma("sp", lambda e: e.dma_start(out=R(xt5, tt)[:], in_=x_d[tt * 128:(tt + 1) * 128, :]), writes=[R(bxt5, tt)])

                def s_gate(tt):
                    S.op("dve", lambda e: e.tensor_tensor(out=R(mTt, tt)[:, 0:4, :], in0=R(mTt, tt)[:, 0:4, :], in1=R(Gt, tt)[:], op=ALU.mult),
                         reads=[R(bmTt, tt), R(bGt, tt)], writes=[R(bmTt, tt)])

                def s_oproj(tt):
                    for hf in range(2):
                        pbi = 2 * (tt % 2) + hf
                        for kc in range(8):
                            S.op("pe", lambda e, hf=hf, kc=kc, pbi=pbi: e.matmul(PB[pbi][:, :], lhsT=R(mTt, tt)[:, kc, :], rhs=wo[:, kc, hf * 512:(hf + 1) * 512],
                                                                             start=(kc == 0), stop=(kc == 7)), reads=[R(bmTt, tt), bwo], writes=[PBb[pbi]])

                def ln_stats(src, bsrc, st6_, bst6_, mv_, bmv_):
                    for hf in range(2):
                        S.op("dve", lambda e, hf=hf: e.bn_stats(out=st6_[:, hf, :], in_=src[:, hf * 512:(hf + 1) * 512]), reads=[bsrc], writes=[bst6_])
                    S.op("dve", lambda e: e.bn_aggr(out=mv_[:, 0:2], in_=st6_[:].rearrange("p a b -> p (a b)")), reads=[bst6_], writes=[bmv_])

                def ln_sqrt(mv_, bmv_):
                    S.op("act", lambda e: e.activation(out=mv_[:, 2:3], in_=mv_[:, 1:2], func=AF.Sqrt, bias=epsT[:, 0:1], scale=1.0), reads=[bmv_, bepsT], writes=[bmv_])

                def ln_recip(mv_, bmv_):
                    S.op("dve", lambda e: e.reciprocal(out=mv_[:, 2:3], in_=mv_[:, 2:3]), reads=[bmv_], writes=[bmv_])
                    S.op("dve", lambda e: e.scalar_tensor_tensor(out=mv_[:, 3:4], in0=mv_[:, 0:1], scalar=-1.0, in1=mv_[:, 2:3], op0=ALU.mult, op1=ALU.mult), reads=[bmv_], writes=[bmv_])

                def s_resid(tt):
                    for hf in range(2):
                        pbi = 2 * (tt % 2) + hf
                        S.op("dve", lambda e, hf=hf, pbi=pbi: e.scalar_tensor_tensor(out=R(t25, tt)[:, hf * 512:(hf + 1) * 512], in0=R(xt5, tt)[:, hf * 512:(hf + 1) * 512], scalar=ALPHA,
                                                                                     in1=PB[pbi][:, :], op0=ALU.mult, op1=ALU.add),
                             reads=[PBb[pbi], R(bxt5, tt)], writes=[R(bt25, tt)])
                    ln_stats(R(t25, tt), R(bt25, tt), R(st6, tt), R(bst6, tt), R(mv, tt), R(bmv, tt))

                def s_sqrt(tt):
                    ln_sqrt(R(mv, tt), R(bmv, tt))

                def s_recip(tt):
                    ln_recip(R(mv, tt), R(bmv, tt))

                def s_norm(tt):
                    S.op("act", lambda e: e.activation(out=R(x15, tt)[:], in_=R(t25, tt)[:], func=AF.Identity, bias=R(mv, tt)[:, 3:4], scale=R(mv, tt)[:, 2:3]),
                         reads=[R(bt25, tt), R(bmv, tt)], writes=[R(bx15, tt)])

                def s_gamma(tt):
                    S.op("pool", lambda e: e.tensor_tensor(out=R(x15, tt)[:], in0=R(x15, tt)[:], in1=ln1g[:], op=ALU.mult), reads=[R(bx15, tt), bln1g], writes=[R(bx15, tt)])

                def s_beta(tt):
                    S.op("dve", lambda e: e.tensor_tensor(out=R(x15, tt)[:], in0=R(x15, tt)[:], in1=ln1b[:], op=ALU.add), reads=[R(bx15, tt), bln1b], writes=[R(bx15, tt)])
                    S.dma("sp", lambda e: e.dma_start(out=X1_d[tt * 128:(tt + 1) * 128, :], in_=R(x15, tt)[:]), reads=[R(bx15, tt)], writes=[bX1], semb=R(bx15, tt))

                def s_h2m(tt):
                    S.op("pool", lambda e: e.tensor_tensor(out=R(h25, tt)[:], in0=R(x15, tt)[:], in1=s2b[:], op=ALU.mult), reads=[R(bx15, tt), bs2b], writes=[R(bh25, tt)])

                def s_h2a(tt):
                    S.op("dve", lambda e: e.tensor_tensor(out=R(h25, tt)[:], in0=R(h25, tt)[:], in1=sh2b[:], op=ALU.add), reads=[R(bh25, tt), bsh2b], writes=[R(bh25, tt)])

                def s_cast(tt):
                    S.op("act", lambda e: e.copy(out=R(h2b, tt)[:], in_=R(h25, tt)[:]), reads=[R(bh25, tt)], writes=[R(bh2b, tt)])
                    S.dma("sp", lambda e: e.dma_start(out=H2_d[tt * 128:(tt + 1) * 128, :], in_=R(h2b, tt)[:]), reads=[R(bh2b, tt)], writes=[bH2], semb=R(bh2b, tt))
                    for kc in range(8):
                        pbi = 4 + kc // 4
                        S.op("pe", lambda e, kc=kc, pbi=pbi: e.transpose(out=PB[pbi][:, (kc % 4) * 128:(kc % 4 + 1) * 128], in_=R(h25, tt)[:, kc * 128:(kc + 1) * 128],
                                                                        identity=cst["ident_f"][:]), reads=[R(bh25, tt), cstb["ident_f"]], writes=[PBb[pbi]])

                def s_h2T(tt):
                    for hf in range(2):
                        S.op("act", lambda e, hf=hf: e.copy(out=R(h2T, tt)[:, hf * 4:(hf + 1) * 4, :], in_=PB[4 + hf][:, :].rearrange("p (c t) -> p c t", c=4)),
                             reads=[PBb[4 + hf]], writes=[R(bh2T, tt)])

                def s_router(tt):
                    pr = 6 + (tt % 2)
                    for kc in range(8):
                        S.op("pe", lambda e, kc=kc: e.matmul(PB[pr][:, 0:36], lhsT=R(h2T, tt)[:, kc, :], rhs=wr[:, kc, :], start=(kc == 0), stop=(kc == 7)),
                             reads=[R(bh2T, tt), bwr], writes=[PBb[pr]])

                def s_rl(tt):
                    pr = 6 + (tt % 2)
                    S.op("dve", lambda e: e.tensor_tensor(out=RL[:, tt, :], in0=PB[pr][:, 0:36], in1=brb[:], op=ALU.add), reads=[PBb[pr], bbrb], writes=[bRL])

                run_pipeline([s_load, s_gate, s_oproj, s_resid, s_sqrt, s_recip, s_norm, s_gamma, s_beta, s_h2m, s_h2a, s_cast, s_h2T, s_router, s_rl], NT)
                S.barrier()
        if stop_after == 5:
            dump(RL[:].rearrange("p t c -> p (t c)"), bRL, NT * 36)

        if stop_after >= 6:
            with ExitStack() as st6_:
                sb6 = lambda n, s_, d: st6_.enter_context(nc.sbuf_tensor(n, s_, d))
                def T6(name, shape, dt=F32):
                    return sb6(name, shape, dt), S.buf(name)
                gmax, bgmax = T6("gmax", [128, NT])
                og, bog = T6("og", [128, NT, 4])
                eg, beg = T6("eg", [128, NT, 4])
                pg, bpg = T6("pg", [128, NT])
                tmp4, btmp4 = T6("tmp4", [128, NT, 4, 8])
                elg, belg = T6("elg", [128, NT, 8])
                m1, bm1 = T6("m1", [128, NT]); m2, bm2 = T6("m2", [128, NT])
                o1, bo1 = T6("o1", [128, NT, 8]); o2, bo2 = T6("o2", [128, NT, 8])
                el2, bel2 = T6("el2", [128, NT, 8])
                ex, bex = T6("ex", [128, NT]); rden, brden = T6("rden", [128, NT])
                sel = [T6("sel%d" % k, [128, NT, 4, 8]) for k in range(2)]
                selS, bselS = T6("selS", [128, NT, 32])
                totS, btotS = T6("totS", [128, NT, 32])
                tpx, btpx = T6("tpx", [128, NT + 1, 32])
                posf, bposf = T6("posf", [128, NT, 32])
                slf = [T6("slf%d" % k, [128, NT]) for k in range(2)]
                gl = RL[:, :, 0:4]
                el = RL[:, :, 4:36].rearrange("p t (g e) -> p t g e", g=4)
                S.op("dve", lambda e: e.tensor_reduce(out=gmax[:], in_=gl, axis=AX.X, op=ALU.max), reads=[bRL], writes=[bgmax])
                S.op("dve", lambda e: e.tensor_tensor(out=og[:], in0=gl, in1=gmax[:].unsqueeze(2).broadcast_to([128, NT, 4]), op=ALU.is_equal), reads=[bRL, bgmax], writes=[bog])
                S.op("dve", lambda e: e.tensor_tensor(out=eg[:], in0=gl, in1=gmax[:].unsqueeze(2).broadcast_to([128, NT, 4]), op=ALU.subtract), reads=[bRL, bgmax], writes=[beg])
                S.op("act", lambda e: e.activation(out=eg[:], in_=eg[:], func=AF.Exp), reads=[beg], writes=[beg])
                S.op("dve", lambda e: e.tensor_reduce(out=pg[:], in_=eg[:], axis=AX.X, op=ALU.add), reads=[beg], writes=[bpg])
                S.op("dve", lambda e: e.reciprocal(out=pg[:], in_=pg[:]), reads=[bpg], writes=[bpg])
                S.op("dve", lambda e: e.tensor_tensor(out=tmp4[:], in0=el, in1=og[:].unsqueeze(3).broadcast_to([128, NT, 4, 8]), op=ALU.mult), reads=[bRL, bog], writes=[btmp4])
                S.op("dve", lambda e: e.tensor_reduce(out=elg[:], in_=tmp4[:].rearrange("p t g e -> p t e g"), axis=AX.X, op=ALU.add), reads=[btmp4], writes=[belg])
                S.op("dve", lambda e: e.tensor_reduce(out=m1[:], in_=elg[:], axis=AX.X, op=ALU.max), reads=[belg], writes=[bm1])
                S.op("dve", lambda e: e.tensor_tensor(out=o1[:], in0=elg[:], in1=m1[:].unsqueeze(2).broadcast_to([128, NT, 8]), op=ALU.is_equal), reads=[belg, bm1], writes=[bo1])
                S.op("dve", lambda e: e.scalar_tensor_tensor(out=el2[:], in0=o1[:], scalar=-1e30, in1=elg[:], op0=ALU.mult, op1=ALU.add), reads=[bo1, belg], writes=[bel2])
                S.op("dve", lambda e: e.tensor_reduce(out=m2[:], in_=el2[:], axis=AX.X, op=ALU.max), reads=[bel2], writes=[bm2])
                S.op("dve", lambda e: e.tensor_tensor(out=o2[:], in0=el2[:], in1=m2[:].unsqueeze(2).broadcast_to([128, NT, 8]), op=ALU.is_equal), reads=[bel2, bm2], writes=[bo2])
                S.op("dve", lambda e: e.tensor_tensor(out=ex[:], in0=m2[:], in1=m1[:], op=ALU.subtract), reads=[bm1, bm2], writes=[bex])
                S.op("act", lambda e: e.activation(out=ex[:], in_=ex[:], func=AF.Exp), reads=[bex], writes=[bex])
                S.op("dve", lambda e: e.tensor_scalar_add(out=rden[:], in0=ex[:], scalar1=1.0), reads=[bex], writes=[brden])
                S.op("dve", lambda e: e.reciprocal(out=rden[:], in_=rden[:]), reads=[brden], writes=[brden])
                S.op("dve", lambda e: e.tensor_tensor(out=wsel[0][:], in0=rden[:], in1=pg[:], op=ALU.mult), reads=[brden, bpg], writes=[bwsel[0]])
                S.op("dve", lambda e: e.tensor_tensor(out=wsel[1][:], in0=wsel[0][:], in1=ex[:], op=ALU.mult), reads=[bwsel[0], bex], writes=[bwsel[1]])
                for k, (ok_, bok_) in enumerate(((o1, bo1), (o2, bo2))):
                    S.op("dve", lambda e, k=k, ok_=ok_: e.tensor_tensor(out=sel[k][0][:], in0=og[:].unsqueeze(3).broadcast_to([128, NT, 4, 8]),
                                                                     in1=ok_[:].unsqueeze(2).broadcast_to([128, NT, 4, 8]), op=ALU.mult), reads=[bog, bok_], writes=[sel[k][1]])
                sel0f = sel[0][0][:].rearrange("p t g e -> p t (g e)")
                sel1f = sel[1][0][:].rearrange("p t g e -> p t (g e)")
                S.op("dve", lambda e: e.tensor_tensor(out=selS[:], in0=sel0f, in1=sel1f, op=ALU.add), reads=[sel[0][1], sel[1][1]], writes=[bselS])
                selSf = selS[:].rearrange("p t e -> p (t e)")
                for hf in range(2):
                    S.op("pe", lambda e, hf=hf: e.matmul(PB[hf][:, :], lhsT=cst["ustrict"][:], rhs=selSf[:, hf * 512:(hf + 1) * 512], start=True, stop=True),
                         reads=[bselS, cstb["ustrict"]], writes=[PBb[hf]])
                    S.op("pe", lambda e, hf=hf: e.matmul(PB[2 + hf][:, :], lhsT=cst["ones_f"][:], rhs=selSf[:, hf * 512:(hf + 1) * 512], start=True, stop=True),
                         reads=[bselS, cstb["ones_f"]], writes=[PBb[2 + hf]])
                    S.op("dve", lambda e, hf=hf: e.tensor_copy(out=totS[:, hf * 16:(hf + 1) * 16, :], in_=PB[2 + hf][:, :].rearrange("p (t e) -> p t e", e=32)),
                         reads=[PBb[2 + hf]], writes=[btotS])
                S.op("pool", lambda e: e.memset(tpx[:, 0, :], 0.0), writes=[btpx])
                for i in range(1, NT + 1):
                    S.op("dve", lambda e, i=i: e.tensor_tensor(out=tpx[:, i, :], in0=tpx[:, i - 1, :], in1=totS[:, i - 1, :], op=ALU.add), reads=[btotS, btpx], writes=[btpx])
                for hf in range(2):
                    S.op("dve", lambda e, hf=hf: e.tensor_tensor(out=posf[:, hf * 16:(hf + 1) * 16, :], in0=PB[hf][:, :].rearrange("p (t e) -> p t e", e=32),
                                                                in1=tpx[:, hf * 16:(hf + 1) * 16, :], op=ALU.add), reads=[PBb[hf], btpx], writes=[bposf])
                key, bkey = T6("key", [128, 32]); rank, brank = T6("rank", [128, 32])
                cmpT, bcmpT = T6("cmpT", [128, 32, 32]); ohR, bohR = T6("ohR", [128, 32, 32])
                base, bbase = T6("base", [128, 32]); capm, bcapm = T6("capm", [128, 32]); eofr, beofr = T6("eofr", [128, 32])
                idxWf, bidxWf = T6("idxWf", [128, 32])
                S.op("dve", lambda e: e.scalar_tensor_tensor(out=key[:], in0=tpx[:, NT, :], scalar=32.0, in1=cst["riota"][:], op0=ALU.mult, op1=ALU.subtract),
                     reads=[btpx, cstb["riota"]], writes=[bkey])
                S.op("dve", lambda e: e.tensor_tensor(out=cmpT[:], in0=key[:].unsqueeze(1).broadcast_to([128, 32, 32]), in1=key[:].unsqueeze(2).broadcast_to([128, 32, 32]), op=ALU.is_gt),
                     reads=[bkey], writes=[bcmpT])
                S.op("dve", lambda e: e.tensor_reduce(out=rank[:], in_=cmpT[:], axis=AX.X, op=ALU.add), reads=[bcmpT], writes=[brank])
                S.op("dve", lambda e: e.tensor_tensor(out=ohR[:], in0=rank[:].unsqueeze(2).broadcast_to([128, 32, 32]), in1=cst["riota"][:].unsqueeze(1).broadcast_to([128, 32, 32]), op=ALU.is_equal),
                     reads=[brank, cstb["riota"]], writes=[bohR])
                S.op("dve", lambda e: e.tensor_tensor(out=cmpT[:], in0=ohR[:], in1=cst["roff"][:].unsqueeze(1).broadcast_to([128, 32, 32]), op=ALU.mult), reads=[bohR, cstb["roff"]], writes=[bcmpT])
                S.op("dve", lambda e: e.tensor_reduce(out=base[:], in_=cmpT[:], axis=AX.X, op=ALU.add), reads=[bcmpT], writes=[bbase])
                S.op("dve", lambda e: e.tensor_tensor(out=cmpT[:], in0=ohR[:], in1=cst["rcap1"][:].unsqueeze(1).broadcast_to([128, 32, 32]), op=ALU.mult), reads=[bohR, cstb["rcap1"]], writes=[bcmpT])
                S.op("dve", lambda e: e.tensor_reduce(out=capm[:], in_=cmpT[:], axis=AX.X, op=ALU.add), reads=[bcmpT], writes=[bcapm])
                S.op("dve", lambda e: e.tensor_tensor(out=cmpT[:], in0=ohR[:].rearrange("p e r -> p r e"), in1=cst["riota"][:].unsqueeze(1).broadcast_to([128, 32, 32]), op=ALU.mult),
                     reads=[bohR, cstb["riota"]], writes=[bcmpT])
                S.op("dve", lambda e: e.tensor_reduce(out=eofr[:], in_=cmpT[:], axis=AX.X, op=ALU.add), reads=[bcmpT], writes=[beofr])
                S.op("dve", lambda e: e.tensor_scalar(out=idxWf[:], in0=eofr[:], scalar1=128.0, scalar2=cst["iotaU"][:, 0:1], op0=ALU.mult, op1=ALU.add),
                     reads=[beofr, cstb["iotaU"]], writes=[bidxWf])
                S.op("dve", lambda e: e.tensor_copy(out=idxW[:], in_=idxWf[:]), reads=[bidxWf], writes=[bidxW])
                S.op("dve", lambda e: e.tensor_tensor(out=posf[:], in0=posf[:], in1=capm[:].unsqueeze(1).broadcast_to([128, NT, 32]), op=ALU.min), reads=[bposf, bcapm], writes=[bposf])
                S.op("dve", lambda e: e.tensor_tensor(out=posf[:], in0=posf[:], in1=base[:].unsqueeze(1).broadcast_to([128, NT, 32]), op=ALU.add), reads=[bposf, bbase], writes=[bposf])
                for k, sf in enumerate((sel0f, sel1f)):
                    S.op("dve", lambda e, k=k, sf=sf: e.tensor_tensor(out=totS[:], in0=sf, in1=posf[:], op=ALU.mult), reads=[sel[k][1], bposf], writes=[btotS])
                    S.op("dve", lambda e, k=k: e.tensor_reduce(out=slf[k][0][:], in_=totS[:], axis=AX.X, op=ALU.add), reads=[btotS], writes=[slf[k][1]])
                    S.op("dve", lambda e, k=k: e.tensor_copy(out=slot_i[k][:], in_=slf[k][0][:]), reads=[slf[k][1]], writes=[bslot[k]])
                if stop_after == 6:
                    dump(slf[0][0][:], slf[0][1], NT)
                    dump(slf[1][0][:], slf[1][1], NT, col0=NT)
                    dump(wsel[0][:], bwsel[0], NT, col0=2 * NT)
                    dump(wsel[1][:], bwsel[1], NT, col0=3 * NT)
                    dump(eofr[:], beofr, 32, col0=4 * NT)
                    dump(base[:], bbase, 32, col0=5 * NT)
                S.barrier()

        if stop_after >= 7:
            with ExitStack() as st7:
                sb7 = lambda n, s_, d: st7.enter_context(nc.sbuf_tensor(n, s_, d))
                hl = [sb7("hl%d" % k, [128, D], BF16) for k in range(2)]; bhl = [S.buf("hl%d" % k) for k in range(2)]
                bsc = [S.buf("scat%d" % k) for k in range(2)]
                for tt in range(NT):
                    k = tt % 2
                    S.dma("sp", lambda e, k=k, tt=tt: e.dma_start(out=hl[k][:], in_=H2_d[tt * 128:(tt + 1) * 128, :]), reads=[bH2], writes=[bhl[k]])
                    for j in range(2):
                        S.dma("pool", lambda e, k=k, tt=tt, j=j: e.indirect_dma_start(
                            out=Xe_d[:, :], out_offset=bass.IndirectOffsetOnAxis(ap=slot_i[j][:, tt:tt + 1], axis=0), in_=hl[k][:], in_offset=None), reads=[bhl[k], bslot[j]], writes=[], semb=bsc[k])
                S.barrier()

        if stop_after >= 7:
            with ExitStack() as st8:
                sb8 = lambda n, s_, d: st8.enter_context(nc.sbuf_tensor(n, s_, d))
                wug = [sb8("wug%d" % k, [128, 8, 1024], BF16) for k in range(3)]; bwu = [S.buf("wug%d" % k) for k in range(3)]
                bwgt = bwu
                wdn = [sb8("wdn%d" % k, [128, 4, D], BF16) for k in range(3)]; bwdn = [S.buf("wdn%d" % k) for k in range(3)]
                xl = [sb8("xl%d" % k, [128, D], BF16) for k in range(2)]; bxl = [S.buf("xl%d" % k) for k in range(2)]
                XeT = [sb8("XeT%d" % k, [128, 8, 512], BF16) for k in range(2)]; bXeT = [S.buf("XeT%d" % k) for k in range(2)]
                sg = [sb8("sg%d" % k, [128, 512], F32) for k in range(2)]; bsg = [S.buf("sg%d" % k) for k in range(2)]
                actT = [sb8("actT%d" % k, [128, 4, 512], BF16) for k in range(2)]; bactT = [S.buf("actT%d" % k) for k in range(2)]
                yb = [sb8("yb%d" % k, [128, D], BF16) for k in range(2)]; byb = [S.buf("yb%d" % k) for k in range(2)]
                chunks = []
                for rg in range(NEXP):
                    o_ = 0
                    first = True
                    while o_ < RCAP[rg]:
                        cs = min(512, RCAP[rg] - o_)
                        chunks.append((rg, o_, cs, first))
                        first = False
                        o_ += cs
                NXT = 3
                XeT3 = XeT + [sb8("XeT2", [128, 8, 512], BF16)]
                bXeT3 = bXeT + [S.buf("XeT2")]
                xl4 = xl + [sb8("xl%d" % k, [128, D], BF16) for k in (2, 3)]
                bxl4 = bxl + [S.buf("xl%d" % k) for k in (2, 3)]
                cnt = {"xi": 0, "yi": 0}

                def load_weights(rg):
                    if rg >= NEXP:
                        return
                    wk_ = rg % 3
                    S.dma("pool", lambda e: e.indirect_dma_start(
                        out=wug[wk_][:].rearrange("p a b -> p (a b)"), out_offset=None, in_=wug_d, in_offset=bass.IndirectOffsetOnAxis(ap=idxW[:, rg:rg + 1], axis=0)),
                        reads=[bidxW], writes=[bwu[wk_]])
                    S.dma("pool", lambda e: e.indirect_dma_start(
                        out=wdn[wk_][:].rearrange("p a b -> p (a b)"), out_offset=None, in_=wdn_d, in_offset=bass.IndirectOffsetOnAxis(ap=idxW[:, rg:rg + 1], axis=0)),
                        reads=[bidxW], writes=[bwdn[wk_]])

                def stage_T(ci):
                    rg, co, cs, first = chunks[ci]
                    if first:
                        load_weights(rg + 1)
                    hk = ci % NXT
                    row0 = ROFF[rg] + co
                    for blk in range(cs // 128):
                        xk = cnt["xi"] % 4
                        pbx = cnt["xi"] % 2
                        cnt["xi"] += 1
                        S.dma("sp", lambda e, xk=xk, blk=blk: e.dma_start(out=xl4[xk][:], in_=Xe_d[row0 + blk * 128:row0 + (blk + 1) * 128, :]), reads=[bXe], writes=[bxl4[xk]])
                        pv = PB[pbx][:].bitcast(BF16)
                        for kc in range(8):
                            S.op("pe", lambda e, xk=xk, kc=kc, pv=pv: e.transpose(out=pv[:, kc * 128:(kc + 1) * 128], in_=xl4[xk][:, kc * 128:(kc + 1) * 128], identity=cst["ident_b"][:]),
                                 reads=[bxl4[xk], cstb["ident_b"]], writes=[PBb[pbx]])
                        if blk % 2 == 0:
                            S.op("dve", lambda e, blk=blk, pv=pv: e.tensor_copy(out=XeT3[hk][:, :, blk * 128:(blk + 1) * 128], in_=pv.rearrange("p (c t) -> p c t", c=8)),
                                 reads=[PBb[pbx]], writes=[bXeT3[hk]])
                        else:
                            S.op("act", lambda e, blk=blk, pv=pv: e.copy(out=XeT3[hk][:, :, blk * 128:(blk + 1) * 128], in_=pv.rearrange("p (c t) -> p c t", c=8)),
                                 reads=[PBb[pbx]], writes=[bXeT3[hk]])

                def stage_U(ci):
                    rg, co, cs, first = chunks[ci]
                    wk_ = rg % 3
                    hk = ci % NXT
                    ak = ci % 2
                    for hc in range(4):
                        pu = 2 + 2 * (hc % 2)
                        pg_ = pu + 1
                        for kc in range(8):
                            S.op("pe", lambda e, hc=hc, kc=kc, pu=pu: e.matmul(PB[pu][:, 0:cs], lhsT=wug[wk_][:, kc, hc * 128:(hc + 1) * 128], rhs=XeT3[hk][:, kc, 0:cs],
                                                                             start=(kc == 0), stop=(kc == 7)), reads=[bwu[wk_], bXeT3[hk]], writes=[PBb[pu]])
                        for kc in range(8):
                            S.op("pe", lambda e, hc=hc, kc=kc, pg_=pg_: e.matmul(PB[pg_][:, 0:cs], lhsT=wug[wk_][:, kc, 512 + hc * 128:512 + (hc + 1) * 128], rhs=XeT3[hk][:, kc, 0:cs],
                                                                               start=(kc == 0), stop=(kc == 7)), reads=[bwgt[wk_], bXeT3[hk]], writes=[PBb[pg_]])
                        sk = hc % 2
                        S.op("act", lambda e, sk=sk, pg_=pg_: e.activation(out=sg[sk][:, 0:cs], in_=PB[pg_][:, 0:cs], func=AF.Silu), reads=[PBb[pg_]], writes=[bsg[sk]])
                        S.op("dve", lambda e, sk=sk, pu=pu, hc=hc: e.tensor_tensor(out=actT[ak][:, hc, 0:cs], in0=sg[sk][:, 0:cs], in1=PB[pu][:, 0:cs], op=ALU.mult),
                             reads=[bsg[sk], PBb[pu]], writes=[bactT[ak]])

                def stage_D(ci):
                    rg, co, cs, first = chunks[ci]
                    wk_ = rg % 3
                    ak = ci % 2
                    row0 = ROFF[rg] + co
                    for blk in range(cs // 128):
                        yk = cnt["yi"] % 2
                        cnt["yi"] += 1
                        for hf in range(2):
                            pbi = 6 + hf
                            for hc in range(4):
                                S.op("pe", lambda e, hc=hc, blk=blk, hf=hf, pbi=pbi: e.matmul(
                                    PB[pbi][:, :], lhsT=actT[ak][:, hc, blk * 128:(blk + 1) * 128], rhs=wdn[wk_][:, hc, hf * 512:(hf + 1) * 512],
                                    start=(hc == 0), stop=(hc == 3)), reads=[bactT[ak], bwdn[wk_]], writes=[PBb[pbi]])
                            if hf == 0:
                                S.op("act", lambda e, yk=yk, pbi=pbi: e.copy(out=yb[yk][:, 0:512], in_=PB[pbi][:, :]), reads=[PBb[pbi]], writes=[byb[yk]])
                            else:
                                S.op("dve", lambda e, yk=yk, pbi=pbi: e.tensor_copy(out=yb[yk][:, 512:1024], in_=PB[pbi][:, :]), reads=[PBb[pbi]], writes=[byb[yk]])
                        S.dma("sp", lambda e, yk=yk, blk=blk: e.dma_start(out=Ys_d[row0 + blk * 128:row0 + (blk + 1) * 128, :], in_=yb[yk][:]), reads=[byb[yk]], writes=[bYs], semb=byb[yk])

                stages = [stage_T, stage_U, stage_D]
                load_weights(0)
                for step in range(len(chunks) + len(stages) - 1):
                    for si in reversed(range(len(stages))):
                        ci = step - si
                        if 0 <= ci < len(chunks):
                            stages[si](ci)
                S.barrier()

        if stop_after >= 8:
            with ExitStack() as st9:
                sb9 = lambda n, s_, d: st9.enter_context(nc.sbuf_tensor(n, s_, d))
                ln2g = sb9("ln2g", [128, D], F32); bln2g = S.buf("ln2g")
                ln2b = sb9("ln2b", [128, D], F32); bln2b = S.buf("ln2b")
                S.dma("sp", lambda e: e.dma_start(out=ln2g[:], in_=ln2g_d.broadcast_to([128, D])), writes=[bln2g])
                S.dma("sp", lambda e: e.dma_start(out=ln2b[:], in_=ln2b_d.broadcast_to([128, D])), writes=[bln2b])

                def ring9(name, shape, dt, n):
                    return [sb9("%s_%d" % (name, k), shape, dt) for k in range(n)], [S.buf("%s_%d" % (name, k)) for k in range(n)]
                x1t, bx1t = ring9("x1t", [128, D], F32, 5)
                Y1, bY1 = ring9("Y1", [128, D], BF16, 3)
                Y2, bY2 = ring9("Y2", [128, D], BF16, 3)
                yy, byy = ring9("yy", [128, D], F32, 7)
                ot, bot = ring9("ot", [128, D], F32, 4)
                st6b, bst6b = ring9("st6b", [128, 2, 6], F32, 6)
                mvb, bmvb = ring9("mvb", [128, 4], F32, 6)

                def R(lst, tt):
                    return lst[tt % len(lst)]

                def e_load(tt):
                    S.dma("sp", lambda e: e.dma_start(out=R(x1t, tt)[:], in_=X1_d[tt * 128:(tt + 1) * 128, :]), reads=[bX1], writes=[R(bx1t, tt)])
                    for (Yt, bYt, j) in ((Y1, bY1, 0), (Y2, bY2, 1)):
                        S.dma("pool", lambda e, j=j, Yt=Yt: e.indirect_dma_start(
                            out=R(Yt, tt)[:], out_offset=None, in_=Ys_d[:, :], in_offset=bass.IndirectOffsetOnAxis(ap=slot_i[j][:, tt:tt + 1], axis=0)),
                            reads=[bYs, bslot[j]], writes=[R(bYt, tt)])

                def e_comb(tt):
                    S.op("act", lambda e: e.activation(out=R(yy, tt)[:], in_=R(Y1, tt)[:], func=AF.Identity, scale=wsel[0][:, tt:tt + 1]),
                         reads=[R(bY1, tt), bwsel[0]], writes=[R(byy, tt)])
                    S.op("dve", lambda e: e.scalar_tensor_tensor(out=R(yy, tt)[:], in0=R(Y2, tt)[:], scalar=wsel[1][:, tt:tt + 1], in1=R(yy, tt)[:], op0=ALU.mult, op1=ALU.add),
                         reads=[R(bY2, tt), bwsel[1], R(byy, tt)], writes=[R(byy, tt)])

                def e_g2(tt):
                    S.op("pool", lambda e: e.tensor_tensor(out=R(yy, tt)[:], in0=R(yy, tt)[:], in1=g2b[:], op=ALU.mult), reads=[R(byy, tt), bg2b], writes=[R(byy, tt)])

                def e_resid(tt):
                    S.op("dve", lambda e: e.scalar_tensor_tensor(out=R(yy, tt)[:], in0=R(x1t, tt)[:], scalar=ALPHA, in1=R(yy, tt)[:], op0=ALU.mult, op1=ALU.add),
                         reads=[R(bx1t, tt), R(byy, tt)], writes=[R(byy, tt)])
                    for hf in range(2):
                        S.op("dve", lambda e, hf=hf: e.bn_stats(out=R(st6b, tt)[:, hf, :], in_=R(yy, tt)[:, hf * 512:(hf + 1) * 512]), reads=[R(byy, tt)], writes=[R(bst6b, tt)])
                    S.op("dve", lambda e: e.bn_aggr(out=R(mvb, tt)[:, 0:2], in_=R(st6b, tt)[:].rearrange("p a b -> p (a b)")), reads=[R(bst6b, tt)], writes=[R(bmvb, tt)])

                def e_sqrt(tt):
                    m_ = R(mvb, tt); bm_ = R(bmvb, tt)
                    S.op("act", lambda e: e.activation(out=m_[:, 2:3], in_=m_[:, 1:2], func=AF.Sqrt, bias=epsT[:, 0:1], scale=1.0), reads=[bm_, bepsT], writes=[bm_])

                def e_recip(tt):
                    m_ = R(mvb, tt); bm_ = R(bmvb, tt)
                    S.op("dve", lambda e: e.reciprocal(out=m_[:, 2:3], in_=m_[:, 2:3]), reads=[bm_], writes=[bm_])
                    S.op("dve", lambda e: e.scalar_tensor_tensor(out=m_[:, 3:4], in0=m_[:, 0:1], scalar=-1.0, in1=m_[:, 2:3], op0=ALU.mult, op1=ALU.mult), reads=[bm_], writes=[bm_])

                def e_norm(tt):
                    S.op("act", lambda e: e.activation(out=R(ot, tt)[:], in_=R(yy, tt)[:], func=AF.Identity, bias=R(mvb, tt)[:, 3:4], scale=R(mvb, tt)[:, 2:3]),
                         reads=[R(byy, tt), R(bmvb, tt)], writes=[R(bot, tt)])

                def e_gamma(tt):
                    S.op("pool", lambda e: e.tensor_tensor(out=R(ot, tt)[:], in0=R(ot, tt)[:], in1=ln2g[:], op=ALU.mult), reads=[R(bot, tt), bln2g], writes=[R(bot, tt)])

                def e_beta(tt):
                    S.op("dve", lambda e: e.tensor_tensor(out=R(ot, tt)[:], in0=R(ot, tt)[:], in1=ln2b[:], op=ALU.add), reads=[R(bot, tt), bln2b], writes=[R(bot, tt)])
                    S.dma("sp", lambda e: e.dma_start(out=out_d[tt * 128:(tt + 1) * 128, :], in_=R(ot, tt)[:]), reads=[R(bot, tt)], writes=[bout], semb=R(bot, tt))

                run_pipeline([e_load, e_comb, e_g2, e_resid, e_sqrt, e_recip, e_norm, e_gamma, e_beta], NT)

        S.barrier()
        with nc.Block() as block:
            S.emit(block)
    return nc


def make_in_maps(inputs):
    cst = host_consts()
    shared = {}
    f32 = lambda a: np.ascontiguousarray(np.asarray(a, dtype=np.float32))
    shared["w_ada"] = f32(inputs["w_ada"])
    shared["b_ada"] = f32(inputs["b_ada"]).reshape(1, -1)
    shared["b_adaT"] = np.ascontiguousarray(f32(inputs["b_ada"]).reshape(48, 128).T)
    shared["w_in"] = f32(inputs["w_in"])
    shared["b_forget"] = f32(inputs["b_forget"]).reshape(1, 8)
    shared["w_out"] = f32(inputs["w_out"])
    for k in ("ln1_g", "ln1_b", "ln2_g", "ln2_b"):
        shared[k] = f32(inputs[k]).reshape(1, D)
    shared["w_router_group"] = f32(inputs["w_router_group"])
    shared["b_router_group"] = f32(inputs["b_router_group"]).reshape(1, 4)
    shared["w_router_expert"] = f32(inputs["w_router_expert"])
    shared["b_router_expert"] = f32(inputs["b_router_expert"]).reshape(1, 32)
    wug = np.concatenate([f32(inputs["w_up"]), f32(inputs["w_gate"])], axis=2)
    shared["w_upgate_r"] = np.ascontiguousarray(wug.reshape(NEXP, 8, 128, 1024).transpose(0, 2, 1, 3)).reshape(NEXP * 128, 8 * 1024)
    shared["w_down_r"] = np.ascontiguousarray(f32(inputs["w_down"]).reshape(NEXP, 4, 128, 1024).transpose(0, 2, 1, 3)).reshape(NEXP * 128, 4 * 1024)
    shared.update(cst)
    x = np.asarray(inputs["x"], dtype=np.float32)
    c = np.asarray(inputs["c"], dtype=np.float32)
    pos = np.asarray(inputs["positions"], dtype=np.int32)
    maps = []
    for b in range(8):
        m = dict(shared)
        m["x"] = np.ascontiguousarray(x[b])
        m["c"] = np.ascontiguousarray(c[b].reshape(8, 128).T)
        m["positions"] = np.ascontiguousarray(pos[b].reshape(1, S_LEN))
        maps.append(m)
    return maps


def kernel(**inputs):
    nc = build_nc()
    maps = make_in_maps(inputs)
    res = run_bass_kernel_spmd(nc, maps, core_ids=list(range(8)))
    out = np.stack([np.asarray(r["out"], dtype=np.float32) for r in res.results], axis=0)
    return out
```

```python
from contextlib import ExitStack
import math
import numpy as np
import ml_dtypes
import concourse.bass as bass
import concourse.mybir as mybir
from concourse.bass_utils import run_bass_kernel_spmd
from concourse.alu_op_type import AluOpType as ALU

F32 = mybir.dt.float32
BF16 = mybir.dt.bfloat16
I32 = mybir.dt.int32
AF = mybir.ActivationFunctionType
AX = mybir.AxisListType

S_LEN = 4096
D = 1024
NT = 32
ALPHA = 2.0 ** 0.25
LN_EPS = 1e-5
NEXP = 32
RCAP = [1152, 896, 768, 768, 640, 640, 640, 640] + [512] * 7 + [384] * 14 + [256] * 3
ROFF = [sum(RCAP[:i]) for i in range(NEXP)]
TOTSLOT = sum(RCAP)


class Buf:
    __slots__ = ("name", "w", "r", "dsem", "dcnt")

    def __init__(self, name):
        self.name = name
        self.w = None
        self.r = []
        self.dsem = None
        self.dcnt = 0


class Sched:
    ENG = ("pe", "act", "dve", "pool", "sp")

    def __init__(self, nc, stack):
        self.nc = nc
        self.stack = stack
        self.prog = {e: [] for e in self.ENG}
        self.sems = {}
        self.cnt = {e: 0 for e in self.ENG}
        self.seen = {e: {} for e in self.ENG}
        for e in self.ENG:
            self.sems[e] = stack.enter_context(nc.semaphore("s_" + e))
        self.nsem = len(self.ENG)
        self.dpool = {}

    def buf(self, name, dgroup=None):
        b = Buf(name)
        if dgroup is not None:
            b.dsem = self._mk(dgroup)
        return b

    def _mk(self, key):
        key = "d_" + key
        if key not in self.sems:
            self.sems[key] = self.stack.enter_context(self.nc.semaphore(key))
            self.nsem += 1
            self.dpool[key] = 0
        return key

    def _dsem(self, b):
        if b.dsem is None:
            b.dsem = self._mk(b.name)
        return b.dsem

    def _waits(self, e, reads, writes):
        toks = []
        for b in reads:
            if b.w is not None:
                toks.append(b.w)
        for b in writes:
            if b.w is not None:
                toks.append(b.w)
            toks.extend(b.r)
        best = {}
        for (k, v) in toks:
            if e == "pe" and k == "pe":
                continue
            if v > best.get(k, 0):
                best[k] = v
        out = []
        for k, v in best.items():
            if self.seen[e].get(k, 0) >= v:
                continue
            self.seen[e][k] = v
            out.append((k, v))
        return out

    def op(self, e, fn, reads=(), writes=()):
        waits = self._waits(e, reads, writes)
        self.cnt[e] += 1
        tok = (e, self.cnt[e])
        self.prog[e].append((waits, fn, (e, 1)))
        for b in reads:
            b.r.append(tok)
            if len(b.r) > 64:
                b.r = b.r[-48:]
        for b in writes:
            b.w = tok
            b.r = []
        return tok

    def dma(self, q, fn, reads=(), writes=(), semb=None):
        waits = self._waits(q, reads, writes)
        sb = semb if semb is not None else (writes[0] if writes else reads[0])
        key = self._dsem(sb)
        self.dpool[key] += 16
        tok = (key, self.dpool[key])
        self.prog[q].append((waits, fn, (key, 16)))
        for b in reads:
            b.r.append(tok)
        for b in writes:
            b.w = tok
            b.r = []
        return tok

    def barrier(self):
        toks = [(e, self.cnt[e]) for e in self.ENG if self.cnt[e] > 0]
        toks += [(k, v) for k, v in self.dpool.items() if v > 0]
        for e in self.ENG:
            waits = []
            for (k, v) in toks:
                if self.seen[e].get(k, 0) >= v:
                    continue
                self.seen[e][k] = v
                waits.append((k, v))
            if waits:
                self.prog[e].append((waits, None, None))

    def final_wait(self, e, bufs):
        waits = self._waits(e, bufs, bufs)
        self.prog[e].append((waits, None, None))

    def emit(self, block):
        sems = self.sems
        prog = self.prog

        def run(eng, lst):
            for waits, fn, inc in lst:
                for (k, v) in waits:
                    eng.wait_ge(sems[k], v)
                if fn is not None:
                    ins = fn(eng)
                    ins.then_inc(sems[inc[0]], inc[1])

        @block.tensor
        def _(eng):
            run(eng, prog["pe"])

        @block.scalar
        def _(eng):
            run(eng, prog["act"])

        @block.vector
        def _(eng):
            run(eng, prog["dve"])

        @block.gpsimd
        def _(eng):
            run(eng, prog["pool"])

        @block.sync
        def _(eng):
            run(eng, prog["sp"])


def host_consts():
    c = {}
    c["ident_f"] = np.eye(128, dtype=np.float32)
    c["ident_b"] = np.eye(128, dtype=np.float32).astype(ml_dtypes.bfloat16)
    j = np.arange(128)
    c["negU"] = -(j[:, None] <= j[None, :]).astype(np.float32)
    c["negOnes"] = -np.ones((128, 128), np.float32)
    c["ustrict"] = (j[:, None] < j[None, :]).astype(np.float32)
    c["ones_f"] = np.ones((128, 128), np.float32)
    mc = (j[:, None] <= j[None, :]).astype(np.float32)
    mp = (j[:, None] >= j[None, :]).astype(np.float32)
    c["mask4"] = np.concatenate([mp, mc, mp, mc], axis=1).astype(ml_dtypes.bfloat16)
    inv = (500000.0 ** (-np.arange(0, 16, 2, dtype=np.float32) / 16.0)).astype(np.float32)
    invf = np.zeros((128, 1), np.float32)
    rot = np.zeros((128, 128), np.float32)
    for hh in range(2):
        for i in range(16):
            invf[hh * 64 + i, 0] = inv[i % 8]
        for i in range(8):
            rot[hh * 64 + i + 8, hh * 64 + i] = -1.0
            rot[hh * 64 + i, hh * 64 + i + 8] = 1.0
    c["invf"] = invf
    bc = lambda v: np.broadcast_to(np.asarray(v, np.float32)[None, :], (128, len(v))).copy()
    c["roff"] = bc(ROFF)
    c["rcap1"] = bc([v - 1 for v in RCAP])
    c["riota"] = bc(np.arange(NEXP))
    p = np.arange(128, dtype=np.float32)[:, None]
    c["iotaU"] = (np.arange(8, dtype=np.float32)[None, :] * 128 + p).astype(np.float32)
    c["iotaD"] = (np.arange(4, dtype=np.float32)[None, :] * 128 + p).astype(np.float32)
    c["rotm"] = rot.astype(ml_dtypes.bfloat16)
    return c


CONST_SPECS = {
    "ident_f": ([128, 128], F32), "ident_b": ([128, 128], BF16), "negU": ([128, 128], F32),
    "negOnes": ([128, 128], F32), "ustrict": ([128, 128], F32), "ones_f": ([128, 128], F32),
    "mask4": ([128, 512], BF16), "invf": ([128, 1], F32), "rotm": ([128, 128], BF16), "roff": ([128, 32], F32), "rcap1": ([128, 32], F32),
    "riota": ([128, 32], F32), "iotaU": ([128, 8], F32), "iotaD": ([128, 4], F32),
}


def build_nc(stop_after=99, debug=False):
    nc = bass.Bass("TRN2", target_bir_lowering=False)
    din = lambda n, s, d=F32: nc.dram_tensor(n, s, d, kind="ExternalInput").ap()
    x_d = din("x", [S_LEN, D])
    c_d = din("c", [128, 8])
    pos_d = din("positions", [1, S_LEN], I32)
    wada_d = din("w_ada", [D, 6 * D])
    bada_d = din("b_ada", [1, 6 * D])
    badaT_d = din("b_adaT", [128, 48])
    win_d = din("w_in", [D, 3592])
    bf_d = din("b_forget", [1, 8])
    wout_d = din("w_out", [D, D])
    ln1g_d = din("ln1_g", [1, D]); ln1b_d = din("ln1_b", [1, D])
    wrg_d = din("w_router_group", [D, 4]); brg_d = din("b_router_group", [1, 4])
    wre_d = din("w_router_expert", [D, 32]); bre_d = din("b_router_expert", [1, 32])
    wug_d = din("w_upgate_r", [NEXP * 128, 8 * 1024])
    wdn_d = din("w_down_r", [NEXP * 128, 4 * 1024])
    ln2g_d = din("ln2_g", [1, D]); ln2b_d = din("ln2_b", [1, D])
    cst_d = {k: din(k, s, d) for k, (s, d) in CONST_SPECS.items()}
    out_d = nc.dram_tensor("out", [S_LEN, D], F32, kind="ExternalOutput").ap()
    dbg_d = nc.dram_tensor("dbg", [128, 8 * S_LEN], F32, kind="ExternalOutput").ap() if debug else None
    scr = lambda n, s, d: nc.dram_tensor(n, s, d, kind="Internal").ap()
    mT_d = scr("mT_scr", [8, 128, S_LEN], BF16)
    G_d = scr("G_scr", [4, 128, S_LEN], BF16)
    vd_d = scr("vd_scr", [4, S_LEN, 2, 65], BF16)

    X1_d = scr("x1_scr", [S_LEN, D], F32)
    H2_d = scr("h2_scr", [S_LEN, D], BF16)
    Xe_d = scr("xe_scr", [TOTSLOT, D], BF16)
    Ys_d = scr("ys_scr", [TOTSLOT, D], BF16)
    win_v = win_d.rearrange("(kc p) c -> p kc c", p=128)

    with ExitStack() as st:
        S = Sched(nc, st)
        sb = lambda n, s, d: st.enter_context(nc.sbuf_tensor(n, s, d))
        PB = [st.enter_context(nc.psum_tensor("pb%d" % i, [128, 512], F32)) for i in range(8)]
        PBb = [S.buf("pb%d" % i) for i in range(8)]
        cst = {}
        cstb = {}
        for k, (s, d) in CONST_SPECS.items():
            cst[k] = sb("c_" + k, s, d)
            cstb[k] = S.buf("c_" + k, dgroup="consts")
            S.dma("sp", lambda e, k=k: e.dma_start(out=cst[k][:], in_=cst_d[k]), writes=[cstb[k]])
        for k in CONST_SPECS:
            cstb[k].w = ("d_consts", S.dpool["d_consts"])
        bout = S.buf("outdram")
        bdbg = S.buf("dbgdram")

        def dump(tile_ap, b, ncols, part=128, col0=0):
            if not debug:
                return
            S.dma("pool", lambda e: e.dma_start(out=dbg_d[0:part, col0:col0 + ncols], in_=tile_ap), reads=[b], writes=[bdbg])

        cT = sb("cT", [128, 8], F32); bcT = S.buf("cT")
        sc = sb("sc", [128, 8], F32); bsc = S.buf("sc")
        scB = sb("scB", [128, 8, 128], F32); bscB = S.buf("scB")
        badaT = sb("badaT", [128, 48], F32); bbadaT = S.buf("badaT")
        modF = sb("modF", [128, 16], F32); bmodF = S.buf("modF")
        g1b = sb("g1b", [128, D], F32); bg1b = S.buf("g1b")
        sh2b = sb("sh2b", [128, D], F32); bsh2b = S.buf("sh2b")
        s2b = sb("s2b", [128, D], F32); bs2b = S.buf("s2b")
        g2b = sb("g2b", [128, D], F32); bg2b = S.buf("g2b")
        S.dma("sp", lambda e: e.dma_start(out=cT[:], in_=c_d), writes=[bcT])
        S.dma("sp", lambda e: e.dma_start(out=badaT[:], in_=badaT_d), writes=[bbadaT])
        S.op("act", lambda e: e.activation(out=sc[:], in_=cT[:], func=AF.Silu), reads=[bcT], writes=[bsc])
        S.op("dve", lambda e: e.tensor_copy(out=scB[:], in_=sc[:].unsqueeze(2).broadcast_to([128, 8, 128])), reads=[bsc], writes=[bscB])
        stA = ExitStack()
        sbA = lambda n, s_, d: stA.enter_context(nc.sbuf_tensor(n, s_, d))
        hT = sbA("hT", [128, 8, S_LEN], BF16)
        bhT = [S.buf("hT%d" % tg) for tg in range(8)]
        with ExitStack() as st0:
            sb0 = lambda n, s, d: st0.enter_context(nc.sbuf_tensor(n, s, d))
            wa = [sb0("wa%d" % i, [128, 8, 512], F32) for i in range(2)]
            bwa = [S.buf("wa%d" % i) for i in range(2)]
            bb = [sb0("bb%d" % i, [128, 512], F32) for i in range(2)]
            bbb = [S.buf("bb%d" % i) for i in range(2)]
            xt = [sb0("xt%d" % i, [128, 4, D], F32) for i in range(2)]
            bxt = [S.buf("xt%d" % i) for i in range(2)]
            wada_v = wada_d.rearrange("(kc p) c -> p kc c", p=128)
            pieces = []
            for i in range(4):
                pieces.append((i * 512, "F", i))
            for vi, dest in ((2, (g1b, bg1b)), (3, (sh2b, bsh2b)), (4, (s2b, bs2b)), (5, (g2b, bg2b))):
                for hf in range(2):
                    pieces.append((vi * 1024 + hf * 512, "B", (dest, hf, vi)))

            def do_piece(pi):
                col0, kind, dest = pieces[pi]
                slot = pi % 2
                q = "sp" if pi % 2 == 0 else "pool"
                for kc4 in range(2):
                    S.dma(q, lambda e, kc4=kc4: e.dma_start(
                        out=wa[slot][:, kc4 * 4:(kc4 + 1) * 4, :], in_=wada_v[:, kc4 * 4:(kc4 + 1) * 4, col0:col0 + 512]), writes=[bwa[slot]])
                pbi = 4 + pi % 2
                if kind == "F":
                    for cc in range(4):
                        col = dest * 4 + cc
                        for kc in range(8):
                            S.op("pe", lambda e, cc=cc, kc=kc, col=col: e.matmul(
                                PB[pbi][:, col:col + 1], lhsT=wa[slot][:, kc, cc * 128:(cc + 1) * 128], rhs=sc[:, kc:kc + 1],
                                start=(kc == 0), stop=(kc == 7)), reads=[bwa[slot], bsc], writes=[PBb[pbi]])
                    S.op("dve", lambda e: e.tensor_tensor(
                        out=modF[:, dest * 4:dest * 4 + 4], in0=PB[pbi][:, dest * 4:dest * 4 + 4], in1=badaT[:, dest * 4:dest * 4 + 4], op=ALU.add),
                        reads=[PBb[pbi], bbadaT], writes=[bmodF])
                else:
                    (dt_, db_), hf, vi = dest
                    S.dma(q, lambda e: e.dma_start(out=bb[slot][:], in_=bada_d[0:1, col0:col0 + 512].broadcast_to([128, 512])), writes=[bbb[slot]])
                    for kc in range(8):
                        S.op("pe", lambda e, kc=kc: e.matmul(
                            PB[pbi][:, :], lhsT=scB[:, kc, :], rhs=wa[slot][:, kc, :], start=(kc == 0), stop=(kc == 7)),
                            reads=[bwa[slot], bscB], writes=[PBb[pbi]])
                    S.op("dve", lambda e: e.tensor_tensor(
                        out=dt_[:, hf * 512:(hf + 1) * 512], in0=PB[pbi][:, :], in1=bb[slot][:], op=ALU.add),
                        reads=[PBb[pbi], bbb[slot]], writes=[db_])

            for pi in range(4):
                do_piece(pi)
            S.op("dve", lambda e: e.tensor_scalar_add(out=modF[:, 8:16], in0=modF[:, 8:16], scalar1=1.0), reads=[bmodF], writes=[bmodF])

            for tg in range(8):
                sl = tg % 2
                xv = x_d[tg * 512:(tg + 1) * 512, :].rearrange("(j p) f -> p j f", p=128)
                for j2 in range(2):
                    S.dma("sp", lambda e, sl=sl, xv=xv, j2=j2: e.dma_start(
                        out=xt[sl][:, j2 * 2:(j2 + 1) * 2, :], in_=xv[:, j2 * 2:(j2 + 1) * 2, :]), writes=[bxt[sl]])
                if stop_after >= 1:
                    for fc in range(8):
                        pbi = (tg * 8 + fc) % 4
                        for j in range(4):
                            S.op("pe", lambda e, sl=sl, fc=fc, j=j, pbi=pbi: e.transpose(
                                out=PB[pbi][:, j * 128:(j + 1) * 128], in_=xt[sl][:, j, fc * 128:(fc + 1) * 128], identity=cst["ident_f"][:]),
                                reads=[bxt[sl], cstb["ident_f"]], writes=[PBb[pbi]])
                        if fc % 2 == 0:
                            S.op("act", lambda e, fc=fc, tg=tg, pbi=pbi: e.activation(
                                out=hT[:, fc, tg * 512:(tg + 1) * 512], in_=PB[pbi][:, :], func=AF.Identity,
                                bias=modF[:, fc:fc + 1], scale=modF[:, 8 + fc:9 + fc]), reads=[PBb[pbi], bmodF], writes=[bhT[tg]])
                        else:
                            S.op("dve", lambda e, fc=fc, tg=tg, pbi=pbi: e.tensor_scalar(
                                out=hT[:, fc, tg * 512:(tg + 1) * 512], in0=PB[pbi][:, :], scalar1=modF[:, 8 + fc:9 + fc],
                                scalar2=modF[:, fc:fc + 1], op0=ALU.mult, op1=ALU.add), reads=[PBb[pbi], bmodF], writes=[bhT[tg]])
                do_piece(4 + tg)
            S.op("dve", lambda e: e.tensor_scalar_add(out=s2b[:], in0=s2b[:], scalar1=1.0), reads=[bs2b], writes=[bs2b])
            S.barrier()
        if stop_after == 0:
            dump(modF[:], bmodF, 16)
            dump(g1b[:], bg1b, 1024, col0=16)
            dump(s2b[:], bs2b, 1024, col0=16 + 1024)
        if stop_after == 1:
            for fc in range(8):
                pass
            tmpd = sb("tmpd", [128, 2048], F32); btmpd = S.buf("tmpd")
            S.op("dve", lambda e: e.tensor_copy(out=tmpd[:, 0:1024].rearrange("p (f t) -> p f t", f=8), in_=hT[:, :, 3072:3200]), reads=bhT, writes=[btmpd])
            S.op("dve", lambda e: e.tensor_copy(out=tmpd[:, 1024:2048], in_=hT[:, 7, 3072:4096]), reads=bhT, writes=[btmpd])
            dump(tmpd[:], btmpd, 2048)

        cumT = sbA("cumT", [128, NT, 8], F32); bcumT = S.buf("cumT")
        tpre = sbA("tpre", [128, NT + 1, 8], F32); btpre = S.buf("tpre")
        if stop_after >= 2:
            with ExitStack() as st2:
                sb2 = lambda n, s, d: st2.enter_context(nc.sbuf_tensor(n, s, d))
                wff = sb2("wff", [128, 8, 8], BF16); bwff = S.buf("wff")
                bfb = sb2("bfb", [128, 8], F32); bbfb = S.buf("bfb")
                zz = sb2("zz", [128, NT, 8], F32); bzz = S.buf("zz")
                tot = sb2("tot", [128, NT, 8], F32); btot = S.buf("tot")
                one1 = sb2("one1", [128, 1], F32); bone1 = S.buf("one1")
                wff_f = sb2("wff_f", [128, 8, 8], F32); bwff_f = S.buf("wff_f")
                S.dma("sp", lambda e: e.dma_start(out=wff_f[:], in_=win_v[:, :, 2048:2056]), writes=[bwff_f])
                S.op("dve", lambda e: e.tensor_copy(out=wff[:], in_=wff_f[:]), reads=[bwff_f], writes=[bwff])
                S.dma("sp", lambda e: e.dma_start(out=bfb[:], in_=bf_d.broadcast_to([128, 8])), writes=[bbfb])
                S.op("pool", lambda e: e.memset(one1[:], 1.0), writes=[bone1])
                for tt in range(NT):
                    for kc in range(8):
                        S.op("pe", lambda e, tt=tt, kc=kc: e.matmul(PB[4][:, tt * 8:(tt + 1) * 8], lhsT=hT[:, kc, tt * 128:(tt + 1) * 128],
                                                                    rhs=wff[:, kc, :], start=(kc == 0), stop=(kc == 7)),
                             reads=[bhT[tt // 4], bwff], writes=[PBb[4]])
                S.op("dve", lambda e: e.tensor_tensor(out=zz[:], in0=PB[4][:, 0:256].rearrange("p (t h) -> p t h", h=8),
                                                      in1=bfb[:].unsqueeze(1).broadcast_to([128, NT, 8]), op=ALU.add),
                     reads=[PBb[4], bbfb], writes=[bzz])
                if stop_after == 2 and debug:
                    dump(zz[:].rearrange("p t h -> p (t h)"), bzz, 256, col0=1024)
                S.op("act", lambda e: e.activation(out=zz[:], in_=zz[:], func=AF.Exp, scale=-1.0), reads=[bzz], writes=[bzz])
                S.op("act", lambda e: e.activation(out=zz[:], in_=zz[:], func=AF.Ln, bias=one1[:, 0:1], scale=1.0), reads=[bzz, bone1], writes=[bzz])
                zf = zz[:].rearrange("p t h -> p (t h)")
                S.op("pe", lambda e: e.matmul(PB[5][:, 0:256], lhsT=cst["negU"][:], rhs=zf, start=True, stop=True), reads=[bzz, cstb["negU"]], writes=[PBb[5]])
                S.op("pe", lambda e: e.matmul(PB[6][:, 0:256], lhsT=cst["negOnes"][:], rhs=zf, start=True, stop=True), reads=[bzz, cstb["negOnes"]], writes=[PBb[6]])
                S.op("dve", lambda e: e.tensor_copy(out=tot[:].rearrange("p t h -> p (t h)"), in_=PB[6][:, 0:256]), reads=[PBb[6]], writes=[btot])
                S.op("pool", lambda e: e.memset(tpre[:, 0, :], 0.0), writes=[btpre])
                for i in range(1, NT + 1):
                    S.op("dve", lambda e, i=i: e.tensor_tensor(out=tpre[:, i, :], in0=tpre[:, i - 1, :], in1=tot[:, i - 1, :], op=ALU.add),
                         reads=[btot, btpre], writes=[btpre])
                S.op("dve", lambda e: e.tensor_tensor(out=cumT[:], in0=PB[5][:, 0:256].rearrange("p (t h) -> p t h", h=8), in1=tpre[:, 0:NT, :], op=ALU.add),
                     reads=[PBb[5], btpre], writes=[bcumT])
                S.barrier()
        if stop_after == 2:
            dump(cumT[:].rearrange("p t h -> p (t h)"), bcumT, 256)
            dump(tpre[:].rearrange("p t h -> p (t h)"), btpre, 264, col0=256)

        cosT = sbA("cosT", [128, S_LEN], BF16); bcos = S.buf("cosT")
        sinT = sbA("sinT", [128, S_LEN], BF16); bsin = S.buf("sinT")
        if stop_after >= 4:
            with ExitStack() as str_:
                sbr = lambda n, s, d: str_.enter_context(nc.sbuf_tensor(n, s, d))
                posi = sbr("posi", [128, S_LEN], I32); bposi = S.buf("posi")
                A_ = sbr("angA", [128, S_LEN], F32); bA = S.buf("angA")
                T_ = sbr("angT", [128, S_LEN], F32); bT = S.buf("angT")
                TWO_PI = 2.0 * math.pi
                C1 = 6.28125
                C2 = TWO_PI - C1
                S.dma("sp", lambda e: e.dma_start(out=posi[:], in_=pos_d.broadcast_to([128, S_LEN])), writes=[bposi])
                S.op("dve", lambda e: e.tensor_copy(out=T_[:], in_=posi[:]), reads=[bposi], writes=[bT])
                S.op("dve", lambda e: e.tensor_scalar(out=A_[:], in0=T_[:], scalar1=cst["invf"][:, 0:1], scalar2=None, op0=ALU.mult), reads=[bT, cstb["invf"]], writes=[bA])

                def reduce_and_sin(dst, bdst, shift):
                    S.op("dve", lambda e: e.tensor_scalar(out=T_[:], in0=A_[:], scalar1=1.0 / TWO_PI, scalar2=shift / TWO_PI, op0=ALU.mult, op1=ALU.add), reads=[bA], writes=[bT])
                    S.op("dve", lambda e: e.tensor_copy(out=posi[:], in_=T_[:]), reads=[bT], writes=[bposi])
                    S.op("dve", lambda e: e.tensor_copy(out=T_[:], in_=posi[:]), reads=[bposi], writes=[bT])
                    U_ = posi[:].bitcast(F32)
                    S.op("dve", lambda e: e.scalar_tensor_tensor(out=U_, in0=T_[:], scalar=-C1, in1=A_[:], op0=ALU.mult, op1=ALU.add), reads=[bT, bA], writes=[bposi])
                    S.op("dve", lambda e: e.scalar_tensor_tensor(out=U_, in0=T_[:], scalar=-C2, in1=U_, op0=ALU.mult, op1=ALU.add), reads=[bT, bposi], writes=[bposi])
                    if shift != 0.0:
                        S.op("dve", lambda e: e.tensor_scalar_add(out=U_, in0=U_, scalar1=shift), reads=[bposi], writes=[bposi])
                    S.op("dve", lambda e: e.tensor_single_scalar(out=T_[:], in_=U_, scalar=math.pi, op=ALU.is_gt), reads=[bposi], writes=[bT])
                    S.op("dve", lambda e: e.scalar_tensor_tensor(out=U_, in0=T_[:], scalar=-TWO_PI, in1=U_, op0=ALU.mult, op1=ALU.add), reads=[bT, bposi], writes=[bposi])
                    S.op("dve", lambda e: e.tensor_single_scalar(out=T_[:], in_=U_, scalar=-math.pi, op=ALU.is_lt), reads=[bposi], writes=[bT])
                    S.op("dve", lambda e: e.scalar_tensor_tensor(out=U_, in0=T_[:], scalar=TWO_PI, in1=U_, op0=ALU.mult, op1=ALU.add), reads=[bT, bposi], writes=[bposi])
                    S.op("act", lambda e: e.activation(out=dst[:], in_=U_, func=AF.Sin), reads=[bposi], writes=[bdst])

                reduce_and_sin(sinT, bsin, 0.0)
                reduce_and_sin(cosT, bcos, math.pi / 2.0)
                S.barrier()
        if stop_after == 4 and debug and False:
            pass

        if stop_after >= 3:
            with ExitStack() as st3:
                sb3 = lambda n, s, d: st3.enter_context(nc.sbuf_tensor(n, s, d))
                wq = sb3("wq", [128, 8, 128], BF16); bwq = S.buf("wq")
                wk = sb3("wk", [128, 8, 128], BF16); bwk = S.buf("wk")
                wv = sb3("wv", [128, 8, 128], BF16); bwv = S.buf("wv")
                wg = sb3("wg", [128, 8, 128], BF16); bwg = S.buf("wg")
                QZ = [sb3("QZ%d" % i, [128, S_LEN], BF16) for i in range(2)]
                bQT = S.buf("QZ")
                bQzero = S.buf("QZzero")
                S.op("pool", lambda e: e.memset(QZ[0][64:128, :], 0.0), writes=[bQzero])
                S.op("pool", lambda e: e.memset(QZ[1][0:64, :], 0.0), writes=[bQT])
                bzx = S.buf("zxe")
                for zi in range(TOTSLOT // 256):
                    S.dma("sp", lambda e, zi=zi: e.dma_start(out=Xe_d[zi * 256:(zi + 1) * 256, :].rearrange("(p j) f -> p (j f)", p=64), in_=QZ[0][64:128, :]),
                          reads=[bQzero], writes=[], semb=bzx)
                KT = sb3("KT", [128, S_LEN], BF16); bKT = S.buf("KT")
                GT = sb3("GT", [128, S_LEN], BF16); bGT = S.buf("GT")
                Va = sb3("Va", [128, NT, 2, 65], BF16); bVa = S.buf("Va")
                biasT = sb3("biasT", [128, 16, NT], F32); bbias = S.buf("biasT")
                Vs = sb3("Vs", [128, 8, 65], BF16); bVs = [S.buf("Vs%d" % i) for i in range(8)]
                PT = [sb3("PT%d" % i, [128, 512], BF16) for i in range(4)]
                bPT = [S.buf("PT%d" % i) for i in range(4)]
                osb = [sb3("osb%d" % i, [65, 256], F32) for i in range(2)]
                bosb = [S.buf("osb%d" % i) for i in range(2)]
                rrow = osb
                brrow = bosb
                stage0 = sb3("stage0", [64, S_LEN], BF16)
                stage = [stage0, stage0]
                bstage0 = S.buf("stage0")
                bstage = [bstage0, bstage0]
                bmT = S.buf("mTdram"); bGd = S.buf("Gdram"); bvd = S.buf("vddram")
                S.op("pool", lambda e: e.memset(Va[:, :, :, 64:65], 1.0), writes=[bVa])

                def load_w(tile, b, col0, q):
                    S.dma("pool", lambda e: e.dma_start(out=tile[:], in_=win_v[:, :, col0:col0 + 128]), writes=[b])

                def proj_fm(wt, bw, evac):
                    for tg in range(8):
                        pbi = tg % 2
                        for kc in range(8):
                            S.op("pe", lambda e, kc=kc, tg=tg, pbi=pbi: e.matmul(PB[pbi][:, :], lhsT=wt[:, kc, :], rhs=hT[:, kc, tg * 512:(tg + 1) * 512],
                                                                              start=(kc == 0), stop=(kc == 7)), reads=[bw, bhT[tg]], writes=[PBb[pbi]])
                        evac(tg, pbi)

                def proj_v(wt, bw):
                    for t4 in range(8):
                        pbi = 2 + (t4 % 2)
                        for j in range(4):
                            tt = t4 * 4 + j
                            for kc in range(8):
                                S.op("pe", lambda e, kc=kc, tt=tt, j=j, pbi=pbi: e.matmul(PB[pbi][:, j * 128:(j + 1) * 128], lhsT=hT[:, kc, tt * 128:(tt + 1) * 128],
                                                                                      rhs=wt[:, kc, :], start=(kc == 0), stop=(kc == 7)),
                                     reads=[bw, bhT[tt // 4]], writes=[PBb[pbi]])
                        S.op("dve", lambda e, t4=t4, pbi=pbi: e.tensor_copy(out=Va[:, t4 * 4:(t4 + 1) * 4, :, 0:64],
                                                                          in_=PB[pbi][:, :].rearrange("p (t h d) -> p t h d", t=4, h=2)),
                             reads=[PBb[pbi]], writes=[bVa])

                def normalize(src_ps_or_sb, bsrc, ncols, dst_ap, bdst, k):
                    o = osb[k]; r = rrow[k]
                    S.op("dve", lambda e: e.tensor_copy(out=o[:, 0:ncols], in_=src_ps_or_sb), reads=[bsrc], writes=[bosb[k]])
                    S.op("dve", lambda e: e.reciprocal(out=r[64:65, 0:ncols], in_=o[64:65, 0:ncols]), reads=[bosb[k]], writes=[brrow[k]])

                    def part2():
                        S.op("pe", lambda e: e.matmul(PB[7][0:64, 0:ncols], lhsT=cst["ones_f"][64:65, 0:64], rhs=r[64:65, 0:ncols], start=True, stop=True),
                             reads=[brrow[k], cstb["ones_f"]], writes=[PBb[7]])
                        S.op("dve", lambda e: e.tensor_tensor(out=dst_ap, in0=o[0:64, 0:ncols], in1=PB[7][0:64, 0:ncols], op=ALU.mult),
                             reads=[bosb[k], PBb[7]], writes=[bdst])
                    return part2

                n_fox = 0 if (stop_after == 4 and debug) else 4
                for pj in range(n_fox):
                    load_w(wq, bwq, 128 * pj, "sp")
                    load_w(wk, bwk, 512 + 128 * pj, "pool")
                    load_w(wv, bwv, 1024 + 128 * pj, "sp")
                    load_w(wg, bwg, 1536 + 128 * pj, "pool")
                    def q_evac(tg, pbi):
                        S.op("act", lambda e: e.copy(out=QZ[0][0:64, tg * 512:(tg + 1) * 512], in_=PB[pbi][0:64, :]), reads=[PBb[pbi]], writes=[bQT])
                        S.op("dve", lambda e: e.tensor_copy(out=QZ[1][64:128, tg * 512:(tg + 1) * 512], in_=PB[pbi][64:128, :]), reads=[PBb[pbi]], writes=[bQT])
                    proj_fm(wq, bwq, q_evac)
                    proj_fm(wk, bwk, lambda tg, pbi: S.op("dve", lambda e: e.tensor_copy(out=KT[:, tg * 512:(tg + 1) * 512], in_=PB[pbi][:, :]), reads=[PBb[pbi]], writes=[bKT]))
                    proj_fm(wg, bwg, lambda tg, pbi: S.op("act", lambda e: e.activation(out=GT[:, tg * 512:(tg + 1) * 512], in_=PB[pbi][:, :], func=AF.Sigmoid), reads=[PBb[pbi]], writes=[bGT]))
                    S.dma("sp", lambda e, pj=pj: e.dma_start(out=G_d[pj], in_=GT[:]), reads=[bGT], writes=[bGd])
                    proj_v(wv, bwv)
                    for hh in range(2):
                        h = 2 * pj + hh
                        d0 = 64 * hh
                        stg = stage[hh]; bstg = bstage[hh]
                        S.op("pool", lambda e: e.memset(biasT[:, :, :], 0.0), writes=[bbias])
                        for g in range(16):
                            S.op("dve", lambda e, g=g, h=h: e.tensor_scalar(out=biasT[:, g, 0:2 * g + 2], in0=cumT[:, 0:2 * g + 2, h], scalar1=-1.0,
                                                                           scalar2=tpre[:, 2 * g, h:h + 1], op0=ALU.mult, op1=ALU.add),
                                 reads=[bcumT, btpre], writes=[bbias])
                        S.op("act", lambda e: e.activation(out=biasT[:, :, :], in_=biasT[:, :, :], func=AF.Exp), reads=[bbias], writes=[bbias])
                        iters = []
                        for g in range(16):
                            nkb = 2 * g + 2
                            for kb in range(0, nkb, 2):
                                iters.append((g, kb, nkb))
                        DEPTH = 3

                        def fox_front(idx, iters=iters, h=h, hh=hh, d0=d0):
                            g, kb, nkb = iters[idx]
                            diag = (kb == 2 * g)
                            n2 = 128 if diag else 256
                            q2 = 128 if diag else 0
                            si = idx % 4
                            vk = (2 * idx) % 8
                            S.op("pe", lambda e: e.matmul(
                                PB[si][:, 0:256], lhsT=KT[:, kb * 128:(kb + 1) * 128], rhs=QZ[hh][:, 256 * g:256 * g + 256],
                                start=True, stop=True), reads=[bKT, bQT, bQzero], writes=[PBb[si]])
                            S.op("pe", lambda e: e.matmul(
                                PB[si][:, 256:256 + n2], lhsT=KT[:, (kb + 1) * 128:(kb + 2) * 128], rhs=QZ[hh][:, 256 * g + q2:256 * g + 256],
                                start=True, stop=True), reads=[bKT, bQT, bQzero], writes=[PBb[si]])
                            S.op("act", lambda e: e.activation(out=PT[si][:, 0:256 + n2], in_=PB[si][:, 0:256 + n2], func=AF.Exp, scale=0.125),
                                 reads=[PBb[si]], writes=[bPT[si]])
                            if diag:
                                S.op("pool", lambda e: e.tensor_tensor(out=PT[si][:, 0:512].rearrange("p (a c) -> p a c", a=2)[:, :, 0:128],
                                                                       in0=PT[si][:, 0:512].rearrange("p (a c) -> p a c", a=2)[:, :, 0:128],
                                                                       in1=cst["mask4"][:, 128:256].unsqueeze(1).broadcast_to([128, 2, 128]), op=ALU.mult),
                                     reads=[bPT[si], cstb["mask4"]], writes=[bPT[si]])
                            S.op("dve", lambda e: e.tensor_tensor(out=Vs[:, vk:vk + 2, :], in0=Va[:, kb:kb + 2, hh, :],
                                                                  in1=biasT[:, g, kb:kb + 2].unsqueeze(2).broadcast_to([128, 2, 65]), op=ALU.mult),
                                 reads=[bVa, bbias], writes=[bVs[vk], bVs[vk + 1]])

                        def fox_back(idx, iters=iters, h=h, hh=hh, d0=d0, stg=stg, bstg=bstg):
                            g, kb, nkb = iters[idx]
                            diag = (kb == 2 * g)
                            n2 = 128 if diag else 256
                            q2 = 128 if diag else 0
                            si = idx % 4
                            vk = (2 * idx) % 8
                            po = 4 + (g % 2)
                            S.op("pe", lambda e: e.matmul(
                                PB[po][0:65, 0:256], lhsT=Vs[:, vk, :], rhs=PT[si][:, 0:256], start=(kb == 0), stop=False),
                                reads=[bVs[vk], bPT[si]], writes=[PBb[po]])
                            S.op("pe", lambda e: e.matmul(
                                PB[po][0:65, q2:256], lhsT=Vs[:, vk + 1, :], rhs=PT[si][:, 256:256 + n2], start=False, stop=(kb + 2 == nkb)),
                                reads=[bVs[vk + 1], bPT[si]], writes=[PBb[po]])
                            if kb + 2 == nkb:
                                pending.append([4, normalize(PB[po][0:65, 0:256], PBb[po], 256, stg[:, 256 * g:256 * (g + 1)], bstg, g % 2)])

                        pending = []
                        for idx in range(len(iters) + DEPTH):
                            if idx < len(iters):
                                fox_front(idx)
                            if idx - DEPTH >= 0:
                                fox_back(idx - DEPTH)
                            for p_ in pending:
                                p_[0] -= 1
                            while pending and pending[0][0] <= 0:
                                pending.pop(0)[1]()
                        while pending:
                            pending.pop(0)[1]()
                        S.dma("sp", lambda e, pj=pj, d0=d0, stg=stg: e.dma_start(out=mT_d[pj, d0:d0 + 64, :], in_=stg[:]), reads=[bstg], writes=[bmT])

                if stop_after >= 4:
                    Vg = {4: sb3("Vg4", [128, NT, 130], BF16), 16: sb3("Vg16", [128, NT, 130], BF16)}
                    bVg = {4: S.buf("Vg4"), 16: S.buf("Vg16")}
                    acc = sb3("acc", [65, S_LEN], F32); bacc = S.buf("acc")
                    qa = [sb3("qa%d" % i, [128, 512], BF16) for i in range(2)]
                    bqa = [S.buf("qa%d" % i) for i in range(2)]
                    S.barrier()
                    GTf = GT[:].bitcast(F32)
                    t1 = [GTf[:, i * 512:(i + 1) * 512] for i in range(2)]
                    bt1 = [S.buf("t1_%d" % i) for i in range(2)]
                    t2 = [GTf[:, (2 + i) * 512:(3 + i) * 512] for i in range(2)]
                    bt2 = [S.buf("t2_%d" % i) for i in range(2)]
                    Vaf = Va[:].rearrange("p t h c -> p t (h c)")

                    def rope_evac(dstT, bdstT):
                        def ev(tg, pbi):
                            k = tg % 2
                            cols = slice(tg * 512, (tg + 1) * 512)
                            S.op("act", lambda e: e.copy(out=qa[k][:], in_=PB[pbi][:, :]), reads=[PBb[pbi]], writes=[bqa[k]])
                            S.op("pe", lambda e: e.matmul(PB[2 + k][:, :], lhsT=cst["rotm"][:], rhs=qa[k][:], start=True, stop=True),
                                 reads=[bqa[k], cstb["rotm"]], writes=[PBb[2 + k]])
                            S.op("dve", lambda e: e.tensor_tensor(out=t1[k], in0=qa[k][:], in1=cosT[:, cols], op=ALU.mult), reads=[bqa[k], bcos], writes=[bt1[k]])
                            S.op("dve", lambda e: e.tensor_tensor(out=t2[k], in0=PB[2 + k][:, :], in1=sinT[:, cols], op=ALU.mult), reads=[PBb[2 + k], bsin], writes=[bt2[k]])
                            if dstT is None:
                                S.op("pool", lambda e: e.tensor_tensor(out=QZ[0][0:64, cols], in0=t1[k][0:64, :], in1=t2[k][0:64, :], op=ALU.add), reads=[bt1[k], bt2[k]], writes=[bdstT])
                                S.op("pool", lambda e: e.tensor_tensor(out=QZ[1][64:128, cols], in0=t1[k][64:128, :], in1=t2[k][64:128, :], op=ALU.add), reads=[bt1[k], bt2[k]], writes=[bdstT])
                            else:
                                S.op("pool", lambda e: e.tensor_tensor(out=dstT[:, cols], in0=t1[k], in1=t2[k], op=ALU.add), reads=[bt1[k], bt2[k]], writes=[bdstT])
                        return ev

                    dil_deferred = []
                    for pj in range(4):
                        load_w(wq, bwq, 2056 + 128 * pj, "pool")
                        load_w(wk, bwk, 2568 + 128 * pj, "pool")
                        load_w(wv, bwv, 3080 + 128 * pj, "pool")
                        proj_fm(wq, bwq, rope_evac(None, bQT))
                        proj_fm(wk, bwk, rope_evac(KT, bKT))
                        proj_v(wv, bwv)
                        S.dma("sp", lambda e, pj=pj: e.dma_start(out=vd_d[pj].rearrange("(t p) h c -> p t (h c)", p=128), in_=Vaf), reads=[bVa], writes=[bvd])
                        for dl in (4, 16):
                            nb = NT // dl
                            src = vd_d[pj].rearrange("(n p r) h c -> p r n (h c)", p=128, r=dl)
                            for r in range(dl):
                                S.dma("sp", lambda e, dl=dl, r=r, nb=nb, src=src: e.dma_start(out=Vg[dl][:, r * nb:(r + 1) * nb, :], in_=src[:, r, :, :]),
                                      reads=[bvd], writes=[bVg[dl]])
                        for hh in range(2):
                            d0 = 64 * hh
                            stg = stage[hh]; bstg = bstage[hh]
                            tiles = []
                            bank_ctr = 0
                            for dl in (1, 4, 16):
                                nb = NT // dl
                                blocks = [(r, n) for r in range(dl) for n in range(nb)]
                                for b0 in range(0, len(blocks), 4):
                                    po = 4 + (bank_ctr % 2)
                                    bank_ctr += 1
                                    runs = []
                                    for bi in range(4):
                                        r, n = blocks[b0 + bi]
                                        if runs and runs[-1][0] == r:
                                            runs[-1][2] += 1
                                        else:
                                            runs.append([r, n, 1, bi])
                                    tiles.append((dl, blocks[b0:b0 + 2], po, 0, None))
                                    tiles.append((dl, blocks[b0 + 2:b0 + 4], po, 256, runs))

                            def vblk(dl, r, n, hh=hh):
                                if dl == 1:
                                    return Va[:, n, hh, :], bVa
                                nb = NT // dl
                                return Vg[dl][:, r * nb + n, hh * 65:(hh + 1) * 65], bVg[dl]

                            def qk(T, dl, r, n, hh=hh):
                                T = KT if T is KT else QZ[hh]
                                st_ = r + dl * 128 * n
                                return T[:, st_:st_ + dl * 127 + 1:dl] if dl > 1 else T[:, st_:st_ + 128]

                            def dil_front(idx, tiles=tiles, qk=qk):
                                dl, blks, po, c0, runs = tiles[idx]
                                si = idx % 4
                                for bl, (r, n) in enumerate(blks):
                                    if n > 0:
                                        S.op("pe", lambda e, r=r, n=n, bl=bl: e.matmul(
                                            PB[si][:, bl * 256:bl * 256 + 128], lhsT=qk(KT, dl, r, n - 1), rhs=qk(None, dl, r, n), start=True, stop=True),
                                            reads=[bKT, bQT, bQzero], writes=[PBb[si]])
                                    S.op("pe", lambda e, r=r, n=n, bl=bl: e.matmul(
                                        PB[si][:, bl * 256 + 128:bl * 256 + 256], lhsT=qk(KT, dl, r, n), rhs=qk(None, dl, r, n), start=True, stop=True),
                                        reads=[bKT, bQT, bQzero], writes=[PBb[si]])
                                S.op("act", lambda e: e.activation(out=PT[si][:, :], in_=PB[si][:, :], func=AF.Exp, scale=0.125),
                                     reads=[PBb[si]], writes=[bPT[si]])
                                meng = "dve" if (idx % 5) in (1, 3) else "pool"
                                S.op(meng, lambda e: e.tensor_tensor(out=PT[si][:, :], in0=PT[si][:, :], in1=cst["mask4"][:, :], op=ALU.mult),
                                     reads=[bPT[si], cstb["mask4"]], writes=[bPT[si]])

                            def dil_back(idx, tiles=tiles, vblk=vblk):
                                dl, blks, po, c0, runs = tiles[idx]
                                si = idx % 4
                                for bl, (r, n) in enumerate(blks):
                                    bc = c0 + bl * 128
                                    if n > 0:
                                        va, bva = vblk(dl, r, n - 1)
                                        S.op("pe", lambda e, va=va, bl=bl, bc=bc: e.matmul(
                                            PB[po][0:65, bc:bc + 128], lhsT=va, rhs=PT[si][:, bl * 256:bl * 256 + 128], start=True, stop=False),
                                            reads=[bva, bPT[si]], writes=[PBb[po]])
                                    va, bva = vblk(dl, r, n)
                                    S.op("pe", lambda e, va=va, bl=bl, bc=bc, n=n: e.matmul(
                                        PB[po][0:65, bc:bc + 128], lhsT=va, rhs=PT[si][:, bl * 256 + 128:bl * 256 + 256], start=(n == 0), stop=True),
                                        reads=[bva, bPT[si]], writes=[PBb[po]])
                                if runs is not None:
                                    for (r, n, cnt_, bi) in runs:
                                        st_ = r + dl * 128 * n
                                        ncol = cnt_ * 128
                                        dst = acc[0:65, st_:st_ + dl * (ncol - 1) + 1:dl] if dl > 1 else acc[0:65, st_:st_ + ncol]
                                        srcp = PB[po][0:65, bi * 128:bi * 128 + ncol]
                                        if dl == 1:
                                            S.op("act", lambda e, dst=dst, srcp=srcp: e.copy(out=dst, in_=srcp), reads=[PBb[po]], writes=[bacc])
                                        else:
                                            S.op("dve", lambda e, dst=dst, srcp=srcp: e.tensor_tensor(out=dst, in0=srcp, in1=dst, op=ALU.add), reads=[PBb[po], bacc], writes=[bacc])

                            DEPTH = 3
                            for idx in range(len(tiles) + DEPTH):
                                if idx < len(tiles):
                                    dil_front(idx)
                                if idx == DEPTH - 1 and dil_deferred:
                                    dil_deferred.pop(0)()
                                if idx - DEPTH >= 0:
                                    dil_back(idx - DEPTH)

                            def norm_job(pj=pj, d0=d0, stg=stg, bstg=bstg):
                                S.op("act", lambda e: e.activation(out=acc[64:65, :], in_=acc[64:65, :], func=AF.Ln), reads=[bacc], writes=[bacc])
                                S.op("act", lambda e: e.activation(out=acc[64:65, :], in_=acc[64:65, :], func=AF.Exp, scale=-1.0), reads=[bacc], writes=[bacc])
                                for cg in range(8):
                                    cols = slice(cg * 512, (cg + 1) * 512)
                                    S.op("pe", lambda e, cols=cols: e.matmul(PB[7][0:64, :], lhsT=cst["ones_f"][64:65, 0:64], rhs=acc[64:65, cols], start=True, stop=True),
                                         reads=[bacc, cstb["ones_f"]], writes=[PBb[7]])
                                    S.op("dve", lambda e, cols=cols: e.tensor_tensor(out=stg[:, cols], in0=acc[0:64, cols], in1=PB[7][0:64, :], op=ALU.mult),
                                         reads=[bacc, PBb[7]], writes=[bstg])
                                S.dma("sp", lambda e: e.dma_start(out=mT_d[4 + pj, d0:d0 + 64, :], in_=stg[:]), reads=[bstg], writes=[bmT])
                            dil_deferred.append(norm_job)
                    while dil_deferred:
                        dil_deferred.pop(0)()

                if stop_after == 4:
                    S.op("dve", lambda e: e.tensor_copy(out=acc[0:64, :], in_=stage[0][:]), reads=[bstage[0]], writes=[bacc])
                    dump(acc[0:64, :], bacc, S_LEN, part=64)
                    S.op("dve", lambda e: e.tensor_copy(out=acc[0:64, :], in_=stage[1][:]), reads=[bstage[1], bdbg], writes=[bacc])
                    dump(acc[0:64, :], bacc, S_LEN, part=64, col0=S_LEN)
                if stop_after == 3:
                    tmpd = sb3("tmpd3", [64, S_LEN], F32); btmpd = S.buf("tmpd3")
                    S.op("dve", lambda e: e.tensor_copy(out=tmpd[:], in_=stage[0][:]), reads=[bstage[0]], writes=[btmpd])
                    dump(tmpd[:], btmpd, S_LEN, part=64)
                    S.op("dve", lambda e: e.tensor_copy(out=tmpd[:], in_=stage[1][:]), reads=[bstage[1], bdbg], writes=[btmpd])
                    dump(tmpd[:], btmpd, S_LEN, part=64, col0=S_LEN)

        S.barrier()
        stA.close()

        RL = sb("RL", [128, NT, 36], F32); bRL = S.buf("RL")
        idxW = sb("idxW", [128, NEXP], I32); bidxW = S.buf("idxW")
        epsT = sb("epsT", [128, 1], F32); bepsT = S.buf("epsT")
        S.op("pool", lambda e: e.memset(epsT[:], LN_EPS), writes=[bepsT])
        slot_i = [sb("slot%d" % k, [128, NT], I32) for k in range(2)]
        bslot = [S.buf("slot%d" % k) for k in range(2)]
        wsel = [sb("wsel%d" % k, [128, NT], F32) for k in range(2)]
        bwsel = [S.buf("wsel%d" % k) for k in range(2)]
        bX1 = S.buf("X1dram"); bH2 = S.buf("H2dram"); bXe = S.buf("Xedram"); bYs = S.buf("Ysdram")

        def layer_norm_tile(src, bsrc, dst, bdst, gam, bgam, bet, bbet, st6, bst6, mv, bmv):
            for hf in range(2):
                S.op("dve", lambda e, hf=hf: e.bn_stats(out=st6[:, hf, :], in_=src[:, hf * 512:(hf + 1) * 512]), reads=[bsrc], writes=[bst6])
            S.op("dve", lambda e: e.bn_aggr(out=mv[:, 0:2], in_=st6[:].rearrange("p a b -> p (a b)")), reads=[bst6], writes=[bmv])
            S.op("act", lambda e: e.activation(out=mv[:, 2:3], in_=mv[:, 1:2], func=AF.Sqrt, bias=epsT[:, 0:1], scale=1.0), reads=[bmv, bepsT], writes=[bmv])
            S.op("dve", lambda e: e.reciprocal(out=mv[:, 2:3], in_=mv[:, 2:3]), reads=[bmv], writes=[bmv])
            S.op("dve", lambda e: e.scalar_tensor_tensor(out=mv[:, 3:4], in0=mv[:, 0:1], scalar=-1.0, in1=mv[:, 2:3], op0=ALU.mult, op1=ALU.mult), reads=[bmv], writes=[bmv])
            S.op("act", lambda e: e.activation(out=dst[:], in_=src[:], func=AF.Identity, bias=mv[:, 3:4], scale=mv[:, 2:3]), reads=[bsrc, bmv], writes=[bdst])
            S.op("pool", lambda e: e.tensor_tensor(out=dst[:], in0=dst[:], in1=gam[:], op=ALU.mult), reads=[bdst, bgam], writes=[bdst])
            S.op("dve", lambda e: e.tensor_tensor(out=dst[:], in0=dst[:], in1=bet[:], op=ALU.add), reads=[bdst, bbet], writes=[bdst])

        def run_pipeline(stages, n):
            for step in range(n + len(stages) - 1):
                for si in reversed(range(len(stages))):
                    t = step - si
                    if 0 <= t < n:
                        stages[si](t)

        if stop_after >= 5:
            with ExitStack() as st5:
                sb5 = lambda n, s_, d: st5.enter_context(nc.sbuf_tensor(n, s_, d))
                wo = sb5("wo", [128, 8, D], BF16); bwo = S.buf("wo")
                ln1g = sb5("ln1g", [128, D], F32); bln1g = S.buf("ln1g")
                ln1b = sb5("ln1b", [128, D], F32); bln1b = S.buf("ln1b")
                wr = sb5("wr", [128, 8, 36], F32); bwr = S.buf("wr")
                brb = sb5("brb", [128, 36], F32); bbrb = S.buf("brb")

                def ring(name, shape, dt, n):
                    return [sb5("%s_%d" % (name, k), shape, dt) for k in range(n)], [S.buf("%s_%d" % (name, k)) for k in range(n)]
                mTt, bmTt = ring("mTt", [128, 8, 128], BF16, 4)
                Gt, bGt = ring("Gt", [128, 4, 128], BF16, 3)
                xt5, bxt5 = ring("xt5", [128, D], F32, 5)
                t25, bt25 = ring("t25", [128, D], F32, 5)
                x15, bx15 = ring("x15", [128, D], F32, 5)
                h25, bh25 = ring("h25", [128, D], F32, 4)
                h2b, bh2b = ring("h2b", [128, D], BF16, 3)
                h2T, bh2T = ring("h2T", [128, 8, 128], F32, 3)
                st6, bst6 = ring("st6", [128, 2, 6], F32, 6)
                mv, bmv = ring("mv", [128, 4], F32, 6)
                wof = sb5("wof", [128, 8, D], F32); bwof = S.buf("wof")
                S.dma("sp", lambda e: e.dma_start(out=wof[:], in_=wout_d.rearrange("(kc p) c -> p kc c", p=128)), writes=[bwof])
                for kc in range(8):
                    S.op("pool" if kc % 2 else "dve", lambda e, kc=kc: e.tensor_tensor(out=wo[:, kc, :], in0=wof[:, kc, :], in1=g1b[:], op=ALU.mult), reads=[bwof, bg1b], writes=[bwo])
                S.dma("sp", lambda e: e.dma_start(out=ln1g[:], in_=ln1g_d.broadcast_to([128, D])), writes=[bln1g])
                S.dma("sp", lambda e: e.dma_start(out=ln1b[:], in_=ln1b_d.broadcast_to([128, D])), writes=[bln1b])
                S.dma("sp", lambda e: e.dma_start(out=wr[:, :, 0:4], in_=wrg_d.rearrange("(kc p) c -> p kc c", p=128)), writes=[bwr])
                S.dma("sp", lambda e: e.dma_start(out=wr[:, :, 4:36], in_=wre_d.rearrange("(kc p) c -> p kc c", p=128)), writes=[bwr])
                S.dma("sp", lambda e: e.dma_start(out=brb[:, 0:4], in_=brg_d.broadcast_to([128, 4])), writes=[bbrb])
                S.dma("sp", lambda e: e.dma_start(out=brb[:, 4:36], in_=bre_d.broadcast_to([128, 32])), writes=[bbrb])

                def R(lst, tt):
                    return lst[tt % len(lst)]

                def s_load(tt):
                    cols = slice(tt * 128, (tt + 1) * 128)
                    S.dma("sp", lambda e: e.dma_start(out=R(mTt, tt)[:], in_=mT_d[:, :, cols].rearrange("c p t -> p c t")), writes=[R(bmTt, tt)])
                    S.dma("sp", lambda e: e.dma_start(out=R(Gt, tt)[:], in_=G_d[:, :, cols].rearrange("c p t -> p c t")), writes=[R(bGt, tt)])
                    S.dma("sp", lambda e: e.dma_start(out=R(xt5, tt)[:], in_=x_d[tt * 128:(tt + 1) * 128, :]), writes=[R(bxt5, tt)])

                def s_gate(tt):
                    S.op("dve", lambda e: e.tensor_tensor(out=R(mTt, tt)[:, 0:4, :], in0=R(mTt, tt)[:, 0:4, :], in1=R(Gt, tt)[:], op=ALU.mult),
                         reads=[R(bmTt, tt), R(bGt, tt)], writes=[R(bmTt, tt)])

                def s_oproj(tt):
                    for hf in range(2):
                        pbi = 2 * (tt % 2) + hf
                        for kc in range(8):
                            S.op("pe", lambda e, hf=hf, kc=kc, pbi=pbi: e.matmul(PB[pbi][:, :], lhsT=R(mTt, tt)[:, kc, :], rhs=wo[:, kc, hf * 512:(hf + 1) * 512],
                                                                             start=(kc == 0), stop=(kc == 7)), reads=[R(bmTt, tt), bwo], writes=[PBb[pbi]])

                def ln_stats(src, bsrc, st6_, bst6_, mv_, bmv_):
                    for hf in range(2):
                        S.op("dve", lambda e, hf=hf: e.bn_stats(out=st6_[:, hf, :], in_=src[:, hf * 512:(hf + 1) * 512]), reads=[bsrc], writes=[bst6_])
                    S.op("dve", lambda e: e.bn_aggr(out=mv_[:, 0:2], in_=st6_[:].rearrange("p a b -> p (a b)")), reads=[bst6_], writes=[bmv_])

                def ln_sqrt(mv_, bmv_):
                    S.op("act", lambda e: e.activation(out=mv_[:, 2:3], in_=mv_[:, 1:2], func=AF.Sqrt, bias=epsT[:, 0:1], scale=1.0), reads=[bmv_, bepsT], writes=[bmv_])

                def ln_recip(mv_, bmv_):
                    S.op("dve", lambda e: e.reciprocal(out=mv_[:, 2:3], in_=mv_[:, 2:3]), reads=[bmv_], writes=[bmv_])
                    S.op("dve", lambda e: e.scalar_tensor_tensor(out=mv_[:, 3:4], in0=mv_[:, 0:1], scalar=-1.0, in1=mv_[:, 2:3], op0=ALU.mult, op1=ALU.mult), reads=[bmv_], writes=[bmv_])

                def s_resid(tt):
                    for hf in range(2):
                        pbi = 2 * (tt % 2) + hf
                        S.op("dve", lambda e, hf=hf, pbi=pbi: e.scalar_tensor_tensor(out=R(t25, tt)[:, hf * 512:(hf + 1) * 512], in0=R(xt5, tt)[:, hf * 512:(hf + 1) * 512], scalar=ALPHA,
                                                                                     in1=PB[pbi][:, :], op0=ALU.mult, op1=ALU.add),
                             reads=[PBb[pbi], R(bxt5, tt)], writes=[R(bt25, tt)])
                    ln_stats(R(t25, tt), R(bt25, tt), R(st6, tt), R(bst6, tt), R(mv, tt), R(bmv, tt))

                def s_sqrt(tt):
                    ln_sqrt(R(mv, tt), R(bmv, tt))

                def s_recip(tt):
                    ln_recip(R(mv, tt), R(bmv, tt))

                def s_norm(tt):
                    S.op("act", lambda e: e.activation(out=R(x15, tt)[:], in_=R(t25, tt)[:], func=AF.Identity, bias=R(mv, tt)[:, 3:4], scale=R(mv, tt)[:, 2:3]),
                         reads=[R(bt25, tt), R(bmv, tt)], writes=[R(bx15, tt)])

                def s_gamma(tt):
                    S.op("pool", lambda e: e.tensor_tensor(out=R(x15, tt)[:], in0=R(x15, tt)[:], in1=ln1g[:], op=ALU.mult), reads=[R(bx15, tt), bln1g], writes=[R(bx15, tt)])

                def s_beta(tt):
                    S.op("dve", lambda e: e.tensor_tensor(out=R(x15, tt)[:], in0=R(x15, tt)[:], in1=ln1b[:], op=ALU.add), reads=[R(bx15, tt), bln1b], writes=[R(bx15, tt)])
                    S.dma("sp", lambda e: e.dma_start(out=X1_d[tt * 128:(tt + 1) * 128, :], in_=R(x15, tt)[:]), reads=[R(bx15, tt)], writes=[bX1], semb=R(bx15, tt))

                def s_h2m(tt):
                    S.op("pool", lambda e: e.tensor_tensor(out=R(h25, tt)[:], in0=R(x15, tt)[:], in1=s2b[:], op=ALU.mult), reads=[R(bx15, tt), bs2b], writes=[R(bh25, tt)])

                def s_h2a(tt):
                    S.op("dve", lambda e: e.tensor_tensor(out=R(h25, tt)[:], in0=R(h25, tt)[:], in1=sh2b[:], op=ALU.add), reads=[R(bh25, tt), bsh2b], writes=[R(bh25, tt)])

                def s_cast(tt):
                    S.op("act", lambda e: e.copy(out=R(h2b, tt)[:], in_=R(h25, tt)[:]), reads=[R(bh25, tt)], writes=[R(bh2b, tt)])
                    S.dma("sp", lambda e: e.dma_start(out=H2_d[tt * 128:(tt + 1) * 128, :], in_=R(h2b, tt)[:]), reads=[R(bh2b, tt)], writes=[bH2], semb=R(bh2b, tt))
                    for kc in range(8):
                        pbi = 4 + kc // 4
                        S.op("pe", lambda e, kc=kc, pbi=pbi: e.transpose(out=PB[pbi][:, (kc % 4) * 128:(kc % 4 + 1) * 128], in_=R(h25, tt)[:, kc * 128:(kc + 1) * 128],
                                                                        identity=cst["ident_f"][:]), reads=[R(bh25, tt), cstb["ident_f"]], writes=[PBb[pbi]])

                def s_h2T(tt):
                    for hf in range(2):
                        S.op("act", lambda e, hf=hf: e.copy(out=R(h2T, tt)[:, hf * 4:(hf + 1) * 4, :], in_=PB[4 + hf][:, :].rearrange("p (c t) -> p c t", c=4)),
                             reads=[PBb[4 + hf]], writes=[R(bh2T, tt)])

                def s_router(tt):
                    pr = 6 + (tt % 2)
                    for kc in range(8):
                        S.op("pe", lambda e, kc=kc: e.matmul(PB[pr][:, 0:36], lhsT=R(h2T, tt)[:, kc, :], rhs=wr[:, kc, :], start=(kc == 0), stop=(kc == 7)),
                             reads=[R(bh2T, tt), bwr], writes=[PBb[pr]])

                def s_rl(tt):
                    pr = 6 + (tt % 2)
                    S.op("dve", lambda e: e.tensor_tensor(out=RL[:, tt, :], in0=PB[pr][:, 0:36], in1=brb[:], op=ALU.add), reads=[PBb[pr], bbrb], writes=[bRL])

                run_pipeline([s_load, s_gate, s_oproj, s_resid, s_sqrt, s_recip, s_norm, s_gamma, s_beta, s_h2m, s_h2a, s_cast, s_h2T, s_router, s_rl], NT)
                S.barrier()
        if stop_after == 5:
            dump(RL[:].rearrange("p t c -> p (t c)"), bRL, NT * 36)

        if stop_after >= 6:
            with ExitStack() as st6_:
                sb6 = lambda n, s_, d: st6_.enter_context(nc.sbuf_tensor(n, s_, d))
                def T6(name, shape, dt=F32):
                    return sb6(name, shape, dt), S.buf(name)
                gmax, bgmax = T6("gmax", [128, NT])
                og, bog = T6("og", [128, NT, 4])
                eg, beg = T6("eg", [128, NT, 4])
                pg, bpg = T6("pg", [128, NT])
                tmp4, btmp4 = T6("tmp4", [128, NT, 4, 8])
                elg, belg = T6("elg", [128, NT, 8])
                m1, bm1 = T6("m1", [128, NT]); m2, bm2 = T6("m2", [128, NT])
                o1, bo1 = T6("o1", [128, NT, 8]); o2, bo2 = T6("o2", [128, NT, 8])
                el2, bel2 = T6("el2", [128, NT, 8])
                ex, bex = T6("ex", [128, NT]); rden, brden = T6("rden", [128, NT])
                sel = [T6("sel%d" % k, [128, NT, 4, 8]) for k in range(2)]
                selS, bselS = T6("selS", [128, NT, 32])
                totS, btotS = T6("totS", [128, NT, 32])
                tpx, btpx = T6("tpx", [128, NT + 1, 32])
                posf, bposf = T6("posf", [128, NT, 32])
                slf = [T6("slf%d" % k, [128, NT]) for k in range(2)]
                gl = RL[:, :, 0:4]
                el = RL[:, :, 4:36].rearrange("p t (g e) -> p t g e", g=4)
                S.op("dve", lambda e: e.tensor_reduce(out=gmax[:], in_=gl, axis=AX.X, op=ALU.max), reads=[bRL], writes=[bgmax])
                S.op("dve", lambda e: e.tensor_tensor(out=og[:], in0=gl, in1=gmax[:].unsqueeze(2).broadcast_to([128, NT, 4]), op=ALU.is_equal), reads=[bRL, bgmax], writes=[bog])
                S.op("dve", lambda e: e.tensor_tensor(out=eg[:], in0=gl, in1=gmax[:].unsqueeze(2).broadcast_to([128, NT, 4]), op=ALU.subtract), reads=[bRL, bgmax], writes=[beg])
                S.op("act", lambda e: e.activation(out=eg[:], in_=eg[:], func=AF.Exp), reads=[beg], writes=[beg])
                S.op("dve", lambda e: e.tensor_reduce(out=pg[:], in_=eg[:], axis=AX.X, op=ALU.add), reads=[beg], writes=[bpg])
                S.op("dve", lambda e: e.reciprocal(out=pg[:], in_=pg[:]), reads=[bpg], writes=[bpg])
                S.op("dve", lambda e: e.tensor_tensor(out=tmp4[:], in0=el, in1=og[:].unsqueeze(3).broadcast_to([128, NT, 4, 8]), op=ALU.mult), reads=[bRL, bog], writes=[btmp4])
                S.op("dve", lambda e: e.tensor_reduce(out=elg[:], in_=tmp4[:].rearrange("p t g e -> p t e g"), axis=AX.X, op=ALU.add), reads=[btmp4], writes=[belg])
                S.op("dve", lambda e: e.tensor_reduce(out=m1[:], in_=elg[:], axis=AX.X, op=ALU.max), reads=[belg], writes=[bm1])
                S.op("dve", lambda e: e.tensor_tensor(out=o1[:], in0=elg[:], in1=m1[:].unsqueeze(2).broadcast_to([128, NT, 8]), op=ALU.is_equal), reads=[belg, bm1], writes=[bo1])
                S.op("dve", lambda e: e.scalar_tensor_tensor(out=el2[:], in0=o1[:], scalar=-1e30, in1=elg[:], op0=ALU.mult, op1=ALU.add), reads=[bo1, belg], writes=[bel2])
                S.op("dve", lambda e: e.tensor_reduce(out=m2[:], in_=el2[:], axis=AX.X, op=ALU.max), reads=[bel2], writes=[bm2])
                S.op("dve", lambda e: e.tensor_tensor(out=o2[:], in0=el2[:], in1=m2[:].unsqueeze(2).broadcast_to([128, NT, 8]), op=ALU.is_equal), reads=[bel2, bm2], writes=[bo2])
                S.op("dve", lambda e: e.tensor_tensor(out=ex[:], in0=m2[:], in1=m1[:], op=ALU.subtract), reads=[bm1, bm2], writes=[bex])
                S.op("act", lambda e: e.activation(out=ex[:], in_=ex[:], func=AF.Exp), reads=[bex], writes=[bex])
                S.op("dve", lambda e: e.tensor_scalar_add(out=rden[:], in0=ex[:], scalar1=1.0), reads=[bex], writes=[brden])
                S.op("dve", lambda e: e.reciprocal(out=rden[:], in_=rden[:]), reads=[brden], writes=[brden])
                S.op("dve", lambda e: e.tensor_tensor(out=wsel[0][:], in0=rden[:], in1=pg[:], op=ALU.mult), reads=[brden, bpg], writes=[bwsel[0]])
                S.op("dve", lambda e: e.tensor_tensor(out=wsel[1][:], in0=wsel[0][:], in1=ex[:], op=ALU.mult), reads=[bwsel[0], bex], writes=[bwsel[1]])
                for k, (ok_, bok_) in enumerate(((o1, bo1), (o2, bo2))):
                    S.op("dve", lambda e, k=k, ok_=ok_: e.tensor_tensor(out=sel[k][0][:], in0=og[:].unsqueeze(3).broadcast_to([128, NT, 4, 8]),
                                                                     in1=ok_[:].unsqueeze(2).broadcast_to([128, NT, 4, 8]), op=ALU.mult), reads=[bog, bok_], writes=[sel[k][1]])
                sel0f = sel[0][0][:].rearrange("p t g e -> p t (g e)")
                sel1f = sel[1][0][:].rearrange("p t g e -> p t (g e)")
                S.op("dve", lambda e: e.tensor_tensor(out=selS[:], in0=sel0f, in1=sel1f, op=ALU.add), reads=[sel[0][1], sel[1][1]], writes=[bselS])
                selSf = selS[:].rearrange("p t e -> p (t e)")
                for hf in range(2):
                    S.op("pe", lambda e, hf=hf: e.matmul(PB[hf][:, :], lhsT=cst["ustrict"][:], rhs=selSf[:, hf * 512:(hf + 1) * 512], start=True, stop=True),
                         reads=[bselS, cstb["ustrict"]], writes=[PBb[hf]])
                    S.op("pe", lambda e, hf=hf: e.matmul(PB[2 + hf][:, :], lhsT=cst["ones_f"][:], rhs=selSf[:, hf * 512:(hf + 1) * 512], start=True, stop=True),
                         reads=[bselS, cstb["ones_f"]], writes=[PBb[2 + hf]])
                    S.op("dve", lambda e, hf=hf: e.tensor_copy(out=totS[:, hf * 16:(hf + 1) * 16, :], in_=PB[2 + hf][:, :].rearrange("p (t e) -> p t e", e=32)),
                         reads=[PBb[2 + hf]], writes=[btotS])
                S.op("pool", lambda e: e.memset(tpx[:, 0, :], 0.0), writes=[btpx])
                for i in range(1, NT + 1):
                    S.op("dve", lambda e, i=i: e.tensor_tensor(out=tpx[:, i, :], in0=tpx[:, i - 1, :], in1=totS[:, i - 1, :], op=ALU.add), reads=[btotS, btpx], writes=[btpx])
                for hf in range(2):
                    S.op("dve", lambda e, hf=hf: e.tensor_tensor(out=posf[:, hf * 16:(hf + 1) * 16, :], in0=PB[hf][:, :].rearrange("p (t e) -> p t e", e=32),
                                                                in1=tpx[:, hf * 16:(hf + 1) * 16, :], op=ALU.add), reads=[PBb[hf], btpx], writes=[bposf])
                key, bkey = T6("key", [128, 32]); rank, brank = T6("rank", [128, 32])
                cmpT, bcmpT = T6("cmpT", [128, 32, 32]); ohR, bohR = T6("ohR", [128, 32, 32])
                base, bbase = T6("base", [128, 32]); capm, bcapm = T6("capm", [128, 32]); eofr, beofr = T6("eofr", [128, 32])
                idxWf, bidxWf = T6("idxWf", [128, 32])
                S.op("dve", lambda e: e.scalar_tensor_tensor(out=key[:], in0=tpx[:, NT, :], scalar=32.0, in1=cst["riota"][:], op0=ALU.mult, op1=ALU.subtract),
                     reads=[btpx, cstb["riota"]], writes=[bkey])
                S.op("dve", lambda e: e.tensor_tensor(out=cmpT[:], in0=key[:].unsqueeze(1).broadcast_to([128, 32, 32]), in1=key[:].unsqueeze(2).broadcast_to([128, 32, 32]), op=ALU.is_gt),
                     reads=[bkey], writes=[bcmpT])
                S.op("dve", lambda e: e.tensor_reduce(out=rank[:], in_=cmpT[:], axis=AX.X, op=ALU.add), reads=[bcmpT], writes=[brank])
                S.op("dve", lambda e: e.tensor_tensor(out=ohR[:], in0=rank[:].unsqueeze(2).broadcast_to([128, 32, 32]), in1=cst["riota"][:].unsqueeze(1).broadcast_to([128, 32, 32]), op=ALU.is_equal),
                     reads=[brank, cstb["riota"]], writes=[bohR])
                S.op("dve", lambda e: e.tensor_tensor(out=cmpT[:], in0=ohR[:], in1=cst["roff"][:].unsqueeze(1).broadcast_to([128, 32, 32]), op=ALU.mult), reads=[bohR, cstb["roff"]], writes=[bcmpT])
                S.op("dve", lambda e: e.tensor_reduce(out=base[:], in_=cmpT[:], axis=AX.X, op=ALU.add), reads=[bcmpT], writes=[bbase])
                S.op("dve", lambda e: e.tensor_tensor(out=cmpT[:], in0=ohR[:], in1=cst["rcap1"][:].unsqueeze(1).broadcast_to([128, 32, 32]), op=ALU.mult), reads=[bohR, cstb["rcap1"]], writes=[bcmpT])
                S.op("dve", lambda e: e.tensor_reduce(out=capm[:], in_=cmpT[:], axis=AX.X, op=ALU.add), reads=[bcmpT], writes=[bcapm])
                S.op("dve", lambda e: e.tensor_tensor(out=cmpT[:], in0=ohR[:].rearrange("p e r -> p r e"), in1=cst["riota"][:].unsqueeze(1).broadcast_to([128, 32, 32]), op=ALU.mult),
                     reads=[bohR, cstb["riota"]], writes=[bcmpT])
                S.op("dve", lambda e: e.tensor_reduce(out=eofr[:], in_=cmpT[:], axis=AX.X, op=ALU.add), reads=[bcmpT], writes=[beofr])
                S.op("dve", lambda e: e.tensor_scalar(out=idxWf[:], in0=eofr[:], scalar1=128.0, scalar2=cst["iotaU"][:, 0:1], op0=ALU.mult, op1=ALU.add),
                     reads=[beofr, cstb["iotaU"]], writes=[bidxWf])
                S.op("dve", lambda e: e.tensor_copy(out=idxW[:], in_=idxWf[:]), reads=[bidxWf], writes=[bidxW])
                S.op("dve", lambda e: e.tensor_tensor(out=posf[:], in0=posf[:], in1=capm[:].unsqueeze(1).broadcast_to([128, NT, 32]), op=ALU.min), reads=[bposf, bcapm], writes=[bposf])
                S.op("dve", lambda e: e.tensor_tensor(out=posf[:], in0=posf[:], in1=base[:].unsqueeze(1).broadcast_to([128, NT, 32]), op=ALU.add), reads=[bposf, bbase], writes=[bposf])
                for k, sf in enumerate((sel0f, sel1f)):
                    S.op("dve", lambda e, k=k, sf=sf: e.tensor_tensor(out=totS[:], in0=sf, in1=posf[:], op=ALU.mult), reads=[sel[k][1], bposf], writes=[btotS])
                    S.op("dve", lambda e, k=k: e.tensor_reduce(out=slf[k][0][:], in_=totS[:], axis=AX.X, op=ALU.add), reads=[btotS], writes=[slf[k][1]])
                    S.op("dve", lambda e, k=k: e.tensor_copy(out=slot_i[k][:], in_=slf[k][0][:]), reads=[slf[k][1]], writes=[bslot[k]])
                if stop_after == 6:
                    dump(slf[0][0][:], slf[0][1], NT)
                    dump(slf[1][0][:], slf[1][1], NT, col0=NT)
                    dump(wsel[0][:], bwsel[0], NT, col0=2 * NT)
                    dump(wsel[1][:], bwsel[1], NT, col0=3 * NT)
                    dump(eofr[:], beofr, 32, col0=4 * NT)
                    dump(base[:], bbase, 32, col0=5 * NT)
                S.barrier()

        if stop_after >= 7:
            with ExitStack() as st7:
                sb7 = lambda n, s_, d: st7.enter_context(nc.sbuf_tensor(n, s_, d))
                hl = [sb7("hl%d" % k, [128, D], BF16) for k in range(2)]; bhl = [S.buf("hl%d" % k) for k in range(2)]
                bsc = [S.buf("scat%d" % k) for k in range(2)]
                for tt in range(NT):
                    k = tt % 2
                    S.dma("sp", lambda e, k=k, tt=tt: e.dma_start(out=hl[k][:], in_=H2_d[tt * 128:(tt + 1) * 128, :]), reads=[bH2], writes=[bhl[k]])
                    for j in range(2):
                        S.dma("pool", lambda e, k=k, tt=tt, j=j: e.indirect_dma_start(
                            out=Xe_d[:, :], out_offset=bass.IndirectOffsetOnAxis(ap=slot_i[j][:, tt:tt + 1], axis=0), in_=hl[k][:], in_offset=None), reads=[bhl[k], bslot[j]], writes=[], semb=bsc[k])
                S.barrier()

        if stop_after >= 7:
            with ExitStack() as st8:
                sb8 = lambda n, s_, d: st8.enter_context(nc.sbuf_tensor(n, s_, d))
                wug = [sb8("wug%d" % k, [128, 8, 1024], BF16) for k in range(3)]; bwu = [S.buf("wug%d" % k) for k in range(3)]
                bwgt = bwu
                wdn = [sb8("wdn%d" % k, [128, 4, D], BF16) for k in range(3)]; bwdn = [S.buf("wdn%d" % k) for k in range(3)]
                xl = [sb8("xl%d" % k, [128, D], BF16) for k in range(2)]; bxl = [S.buf("xl%d" % k) for k in range(2)]
                XeT = [sb8("XeT%d" % k, [128, 8, 512], BF16) for k in range(2)]; bXeT = [S.buf("XeT%d" % k) for k in range(2)]
                sg = [sb8("sg%d" % k, [128, 512], F32) for k in range(2)]; bsg = [S.buf("sg%d" % k) for k in range(2)]
                actT = [sb8("actT%d" % k, [128, 4, 512], BF16) for k in range(2)]; bactT = [S.buf("actT%d" % k) for k in range(2)]
                yb = [sb8("yb%d" % k, [128, D], BF16) for k in range(2)]; byb = [S.buf("yb%d" % k) for k in range(2)]
                chunks = []
                for rg in range(NEXP):
                    o_ = 0
                    first = True
                    while o_ < RCAP[rg]:
                        cs = min(512, RCAP[rg] - o_)
                        chunks.append((rg, o_, cs, first))
                        first = False
                        o_ += cs
                NXT = 3
                XeT3 = XeT + [sb8("XeT2", [128, 8, 512], BF16)]
                bXeT3 = bXeT + [S.buf("XeT2")]
                xl4 = xl + [sb8("xl%d" % k, [128, D], BF16) for k in (2, 3)]
                bxl4 = bxl + [S.buf("xl%d" % k) for k in (2, 3)]
                cnt = {"xi": 0, "yi": 0}

                def load_weights(rg):
                    if rg >= NEXP:
                        return
                    wk_ = rg % 3
                    S.dma("pool", lambda e: e.indirect_dma_start(
                        out=wug[wk_][:].rearrange("p a b -> p (a b)"), out_offset=None, in_=wug_d, in_offset=bass.IndirectOffsetOnAxis(ap=idxW[:, rg:rg + 1], axis=0)),
                        reads=[bidxW], writes=[bwu[wk_]])
                    S.dma("pool", lambda e: e.indirect_dma_start(
                        out=wdn[wk_][:].rearrange("p a b -> p (a b)"), out_offset=None, in_=wdn_d, in_offset=bass.IndirectOffsetOnAxis(ap=idxW[:, rg:rg + 1], axis=0)),
                        reads=[bidxW], writes=[bwdn[wk_]])

                def stage_T(ci):
                    rg, co, cs, first = chunks[ci]
                    if first:
                        load_weights(rg + 1)
                    hk = ci % NXT
                    row0 = ROFF[rg] + co
                    for blk in range(cs // 128):
                        xk = cnt["xi"] % 4
                        pbx = cnt["xi"] % 2
                        cnt["xi"] += 1
                        S.dma("sp", lambda e, xk=xk, blk=blk: e.dma_start(out=xl4[xk][:], in_=Xe_d[row0 + blk * 128:row0 + (blk + 1) * 128, :]), reads=[bXe], writes=[bxl4[xk]])
                        pv = PB[pbx][:].bitcast(BF16)
                        for kc in range(8):
                            S.op("pe", lambda e, xk=xk, kc=kc, pv=pv: e.transpose(out=pv[:, kc * 128:(kc + 1) * 128], in_=xl4[xk][:, kc * 128:(kc + 1) * 128], identity=cst["ident_b"][:]),
                                 reads=[bxl4[xk], cstb["ident_b"]], writes=[PBb[pbx]])
                        if blk % 2 == 0:
                            S.op("dve", lambda e, blk=blk, pv=pv: e.tensor_copy(out=XeT3[hk][:, :, blk * 128:(blk + 1) * 128], in_=pv.rearrange("p (c t) -> p c t", c=8)),
                                 reads=[PBb[pbx]], writes=[bXeT3[hk]])
                        else:
                            S.op("act", lambda e, blk=blk, pv=pv: e.copy(out=XeT3[hk][:, :, blk * 128:(blk + 1) * 128], in_=pv.rearrange("p (c t) -> p c t", c=8)),
                                 reads=[PBb[pbx]], writes=[bXeT3[hk]])

                def stage_U(ci):
                    rg, co, cs, first = chunks[ci]
                    wk_ = rg % 3
                    hk = ci % NXT
                    ak = ci % 2
                    for hc in range(4):
                        pu = 2 + 2 * (hc % 2)
                        pg_ = pu + 1
                        for kc in range(8):
                            S.op("pe", lambda e, hc=hc, kc=kc, pu=pu: e.matmul(PB[pu][:, 0:cs], lhsT=wug[wk_][:, kc, hc * 128:(hc + 1) * 128], rhs=XeT3[hk][:, kc, 0:cs],
                                                                             start=(kc == 0), stop=(kc == 7)), reads=[bwu[wk_], bXeT3[hk]], writes=[PBb[pu]])
                        for kc in range(8):
                            S.op("pe", lambda e, hc=hc, kc=kc, pg_=pg_: e.matmul(PB[pg_][:, 0:cs], lhsT=wug[wk_][:, kc, 512 + hc * 128:512 + (hc + 1) * 128], rhs=XeT3[hk][:, kc, 0:cs],
                                                                               start=(kc == 0), stop=(kc == 7)), reads=[bwgt[wk_], bXeT3[hk]], writes=[PBb[pg_]])
                        sk = hc % 2
                        S.op("act", lambda e, sk=sk, pg_=pg_: e.activation(out=sg[sk][:, 0:cs], in_=PB[pg_][:, 0:cs], func=AF.Silu), reads=[PBb[pg_]], writes=[bsg[sk]])
                        S.op("dve", lambda e, sk=sk, pu=pu, hc=hc: e.tensor_tensor(out=actT[ak][:, hc, 0:cs], in0=sg[sk][:, 0:cs], in1=PB[pu][:, 0:cs], op=ALU.mult),
                             reads=[bsg[sk], PBb[pu]], writes=[bactT[ak]])

                def stage_D(ci):
                    rg, co, cs, first = chunks[ci]
                    wk_ = rg % 3
                    ak = ci % 2
                    row0 = ROFF[rg] + co
                    for blk in range(cs // 128):
                        yk = cnt["yi"] % 2
                        cnt["yi"] += 1
                        for hf in range(2):
                            pbi = 6 + hf
                            for hc in range(4):
                                S.op("pe", lambda e, hc=hc, blk=blk, hf=hf, pbi=pbi: e.matmul(
                                    PB[pbi][:, :], lhsT=actT[ak][:, hc, blk * 128:(blk + 1) * 128], rhs=wdn[wk_][:, hc, hf * 512:(hf + 1) * 512],
                                    start=(hc == 0), stop=(hc == 3)), reads=[bactT[ak], bwdn[wk_]], writes=[PBb[pbi]])
                            if hf == 0:
                                S.op("act", lambda e, yk=yk, pbi=pbi: e.copy(out=yb[yk][:, 0:512], in_=PB[pbi][:, :]), reads=[PBb[pbi]], writes=[byb[yk]])
                            else:
                                S.op("dve", lambda e, yk=yk, pbi=pbi: e.tensor_copy(out=yb[yk][:, 512:1024], in_=PB[pbi][:, :]), reads=[PBb[pbi]], writes=[byb[yk]])
                        S.dma("sp", lambda e, yk=yk, blk=blk: e.dma_start(out=Ys_d[row0 + blk * 128:row0 + (blk + 1) * 128, :], in_=yb[yk][:]), reads=[byb[yk]], writes=[bYs], semb=byb[yk])

                stages = [stage_T, stage_U, stage_D]
                load_weights(0)
                for step in range(len(chunks) + len(stages) - 1):
                    for si in reversed(range(len(stages))):
                        ci = step - si
                        if 0 <= ci < len(chunks):
                            stages[si](ci)
                S.barrier()

        if stop_after >= 8:
            with ExitStack() as st9:
                sb9 = lambda n, s_, d: st9.enter_context(nc.sbuf_tensor(n, s_, d))
                ln2g = sb9("ln2g", [128, D], F32); bln2g = S.buf("ln2g")
                ln2b = sb9("ln2b", [128, D], F32); bln2b = S.buf("ln2b")
                S.dma("sp", lambda e: e.dma_start(out=ln2g[:], in_=ln2g_d.broadcast_to([128, D])), writes=[bln2g])
                S.dma("sp", lambda e: e.dma_start(out=ln2b[:], in_=ln2b_d.broadcast_to([128, D])), writes=[bln2b])

                def ring9(name, shape, dt, n):
                    return [sb9("%s_%d" % (name, k), shape, dt) for k in range(n)], [S.buf("%s_%d" % (name, k)) for k in range(n)]
                x1t, bx1t = ring9("x1t", [128, D], F32, 5)
                Y1, bY1 = ring9("Y1", [128, D], BF16, 3)
                Y2, bY2 = ring9("Y2", [128, D], BF16, 3)
                yy, byy = ring9("yy", [128, D], F32, 7)
                ot, bot = ring9("ot", [128, D], F32, 4)
                st6b, bst6b = ring9("st6b", [128, 2, 6], F32, 6)
                mvb, bmvb = ring9("mvb", [128, 4], F32, 6)

                def R(lst, tt):
                    return lst[tt % len(lst)]

                def e_load(tt):
                    S.dma("sp", lambda e: e.dma_start(out=R(x1t, tt)[:], in_=X1_d[tt * 128:(tt + 1) * 128, :]), reads=[bX1], writes=[R(bx1t, tt)])
                    for (Yt, bYt, j) in ((Y1, bY1, 0), (Y2, bY2, 1)):
                        S.dma("pool", lambda e, j=j, Yt=Yt: e.indirect_dma_start(
                            out=R(Yt, tt)[:], out_offset=None, in_=Ys_d[:, :], in_offset=bass.IndirectOffsetOnAxis(ap=slot_i[j][:, tt:tt + 1], axis=0)),
                            reads=[bYs, bslot[j]], writes=[R(bYt, tt)])

                def e_comb(tt):
                    S.op("act", lambda e: e.activation(out=R(yy, tt)[:], in_=R(Y1, tt)[:], func=AF.Identity, scale=wsel[0][:, tt:tt + 1]),
                         reads=[R(bY1, tt), bwsel[0]], writes=[R(byy, tt)])
                    S.op("dve", lambda e: e.scalar_tensor_tensor(out=R(yy, tt)[:], in0=R(Y2, tt)[:], scalar=wsel[1][:, tt:tt + 1], in1=R(yy, tt)[:], op0=ALU.mult, op1=ALU.add),
                         reads=[R(bY2, tt), bwsel[1], R(byy, tt)], writes=[R(byy, tt)])

                def e_g2(tt):
                    S.op("dve", lambda e: e.tensor_tensor(out=R(yy, tt)[:], in0=R(yy, tt)[:], in1=g2b[:], op=ALU.mult), reads=[R(byy, tt), bg2b], writes=[R(byy, tt)])

                def e_resid(tt):
                    S.op("dve", lambda e: e.scalar_tensor_tensor(out=R(yy, tt)[:], in0=R(x1t, tt)[:], scalar=ALPHA, in1=R(yy, tt)[:], op0=ALU.mult, op1=ALU.add),
                         reads=[R(bx1t, tt), R(byy, tt)], writes=[R(byy, tt)])
                    for hf in range(2):
                        S.op("dve", lambda e, hf=hf: e.bn_stats(out=R(st6b, tt)[:, hf, :], in_=R(yy, tt)[:, hf * 512:(hf + 1) * 512]), reads=[R(byy, tt)], writes=[R(bst6b, tt)])
                    S.op("dve", lambda e: e.bn_aggr(out=R(mvb, tt)[:, 0:2], in_=R(st6b, tt)[:].rearrange("p a b -> p (a b)")), reads=[R(bst6b, tt)], writes=[R(bmvb, tt)])

                def e_sqrt(tt):
                    m_ = R(mvb, tt); bm_ = R(bmvb, tt)
                    S.op("act", lambda e: e.activation(out=m_[:, 2:3], in_=m_[:, 1:2], func=AF.Sqrt, bias=epsT[:, 0:1], scale=1.0), reads=[bm_, bepsT], writes=[bm_])

                def e_recip(tt):
                    m_ = R(mvb, tt); bm_ = R(bmvb, tt)
                    S.op("dve", lambda e: e.reciprocal(out=m_[:, 2:3], in_=m_[:, 2:3]), reads=[bm_], writes=[bm_])
                    S.op("dve", lambda e: e.scalar_tensor_tensor(out=m_[:, 3:4], in0=m_[:, 0:1], scalar=-1.0, in1=m_[:, 2:3], op0=ALU.mult, op1=ALU.mult), reads=[bm_], writes=[bm_])

                def e_norm(tt):
                    S.op("act", lambda e: e.activation(out=R(ot, tt)[:], in_=R(yy, tt)[:], func=AF.Identity, bias=R(mvb, tt)[:, 3:4], scale=R(mvb, tt)[:, 2:3]),
                         reads=[R(byy, tt), R(bmvb, tt)], writes=[R(bot, tt)])

                def e_gamma(tt):
                    S.op("pool", lambda e: e.tensor_tensor(out=R(ot, tt)[:], in0=R(ot, tt)[:], in1=ln2g[:], op=ALU.mult), reads=[R(bot, tt), bln2g], writes=[R(bot, tt)])

                def e_beta(tt):
                    S.op("dve", lambda e: e.tensor_tensor(out=R(ot, tt)[:], in0=R(ot, tt)[:], in1=ln2b[:], op=ALU.add), reads=[R(bot, tt), bln2b], writes=[R(bot, tt)])
                    S.dma("sp", lambda e: e.dma_start(out=out_d[tt * 128:(tt + 1) * 128, :], in_=R(ot, tt)[:]), reads=[R(bot, tt)], writes=[bout], semb=R(bot, tt))

                run_pipeline([e_load, e_comb, e_g2, e_resid, e_sqrt, e_recip, e_norm, e_gamma, e_beta], NT)

        S.barrier()
        with nc.Block() as block:
            S.emit(block)
    return nc


def make_in_maps(inputs):
    cst = host_consts()
    shared = {}
    f32 = lambda a: np.ascontiguousarray(np.asarray(a, dtype=np.float32))
    shared["w_ada"] = f32(inputs["w_ada"])
    shared["b_ada"] = f32(inputs["b_ada"]).reshape(1, -1)
    shared["b_adaT"] = np.ascontiguousarray(f32(inputs["b_ada"]).reshape(48, 128).T)
    shared["w_in"] = f32(inputs["w_in"])
    shared["b_forget"] = f32(inputs["b_forget"]).reshape(1, 8)
    shared["w_out"] = f32(inputs["w_out"])
    for k in ("ln1_g", "ln1_b", "ln2_g", "ln2_b"):
        shared[k] = f32(inputs[k]).reshape(1, D)
    shared["w_router_group"] = f32(inputs["w_router_group"])
    shared["b_router_group"] = f32(inputs["b_router_group"]).reshape(1, 4)
    shared["w_router_expert"] = f32(inputs["w_router_expert"])
    shared["b_router_expert"] = f32(inputs["b_router_expert"]).reshape(1, 32)
    wug = np.concatenate([f32(inputs["w_up"]), f32(inputs["w_gate"])], axis=2)
    shared["w_upgate_r"] = np.ascontiguousarray(wug.reshape(NEXP, 8, 128, 1024).transpose(0, 2, 1, 3)).reshape(NEXP * 128, 8 * 1024)
    shared["w_down_r"] = np.ascontiguousarray(f32(inputs["w_down"]).reshape(NEXP, 4, 128, 1024).transpose(0, 2, 1, 3)).reshape(NEXP * 128, 4 * 1024)
    shared.update(cst)
    x = np.asarray(inputs["x"], dtype=np.float32)
    c = np.asarray(inputs["c"], dtype=np.float32)
    pos = np.asarray(inputs["positions"], dtype=np.int32)
    maps = []
    for b in range(8):
        m = dict(shared)
        m["x"] = np.ascontiguousarray(x[b])
        m["c"] = np.ascontiguousarray(c[b].reshape(8, 128).T)
        m["positions"] = np.ascontiguousarray(pos[b].reshape(1, S_LEN))
        maps.append(m)
    return maps


def kernel(**inputs):
    nc = build_nc()
    maps = make_in_maps(inputs)
    res = run_bass_kernel_spmd(nc, maps, core_ids=list(range(8)))
    out = np.stack([np.asarray(r["out"], dtype=np.float32) for r in res.results], axis=0)
    return out
```
